# Optimizing a Trainium2 kernel written in Bass

```python
import math
import jax, jax.numpy as jnp
from jax import lax
import numpy as np

D_MODEL = 1024
BATCH = 8
SEQ = 2048
DEPTH = 2

N_A = DEPTH // 2
N_B = DEPTH - N_A
HEAD_DIM = 64
CHUNK = 128
N_SG = 12
SG_WIDTH = N_SG * HEAD_DIM
DIL_PAIRS = ((128, 1), (512, 4), (2048, 16))
HEADS_PER_DIL = 4
N_DIL_HEADS = HEADS_PER_DIL * len(DIL_PAIRS)
DIL_Q_WIDTH = N_DIL_HEADS * HEAD_DIM
DIL_OUT_WIDTH = HEADS_PER_DIL * HEAD_DIM
N_MEM = 256
MEM_HEADS = 4
MEM_WIDTH = MEM_HEADS * HEAD_DIM
N_EXPERTS = 16
N_EXPERT_GROUPS = 4
EXPERTS_PER_GROUP = N_EXPERTS // N_EXPERT_GROUPS
TOP_K = 2
D_EXPERT = 1024
MOE_BLOCK = 128
ALPHA = (2 * DEPTH) ** 0.25
BETA = (8 * DEPTH) ** -0.25
LN_EPS = 1e-5
ATT_SCALE = 1.0 / math.sqrt(HEAD_DIM)

kernel_name = "yoco_gmlp_dilated_moe_trunk"


def layer_norm(x, g, b):
    xf = x.astype(jnp.float32)
    mu = jnp.mean(xf, -1, keepdims=True)
    var = jnp.mean(jnp.square(xf - mu), -1, keepdims=True)
    return ((xf - mu) * lax.rsqrt(var + LN_EPS)).astype(x.dtype) * g + b


def alibi_slopes():
    return 2.0 ** (-8.0 * jnp.arange(1, N_DIL_HEADS + 1, dtype=jnp.float32) / N_DIL_HEADS)


def spatial_gating(u, v, w_s, b_s):
    B, S, G, Dh = v.shape
    vc = v.reshape(B, S // CHUNK, CHUNK, G, Dh)
    causal = jnp.tril(jnp.ones((CHUNK, CHUNK), bool))
    w = jnp.where(causal[None], w_s, 0).astype(v.dtype)
    s = jnp.einsum('gpq,bcqgd->bcpgd', w, vc) + b_s.T[None, None, :, :, None].astype(v.dtype)
    return u * s.reshape(B, S, G, Dh)


def dilated_branch(q, k, v, window, dilation, slopes):
    B, S, H, Dh = q.shape
    steps = window // dilation
    L = S // dilation
    N = B * dilation

    def by_residue(t):
        return t.reshape(B, L, dilation, H, Dh).transpose(0, 2, 3, 1, 4).reshape(N, H, L, Dh)

    qr, kr, vr = by_residue(q), by_residue(k), by_residue(v)
    nb = -(-L // CHUNK)
    pad = nb * CHUNK - L
    qr = jnp.pad(qr, ((0, 0), (0, 0), (0, pad), (0, 0)))
    kr = jnp.pad(kr, ((0, 0), (0, 0), (CHUNK, pad), (0, 0)))
    vr = jnp.pad(vr, ((0, 0), (0, 0), (CHUNK, pad), (0, 0)))
    qb = qr.reshape(N, H, nb, CHUNK, Dh)
    kb = kr.reshape(N, H, nb + 1, CHUNK, Dh)
    vb = vr.reshape(N, H, nb + 1, CHUNK, Dh)
    kband = jnp.concatenate([kb[:, :, :-1], kb[:, :, 1:]], axis=3)
    vband = jnp.concatenate([vb[:, :, :-1], vb[:, :, 1:]], axis=3)
    s = jnp.einsum('nhbqd,nhbkd->nhbqk', qb, kband).astype(jnp.float32) * ATT_SCALE
    iq = jnp.arange(CHUNK)[:, None]
    jk = jnp.arange(2 * CHUNK)[None, :]
    rel = CHUNK + iq - jk
    key_pos = (jnp.arange(nb)[:, None, None] - 1) * CHUNK + jk[None]
    valid = (rel >= 0)[None] & (rel <= steps)[None] & (key_pos >= 0)
    bias = -slopes[:, None, None, None] * (dilation * rel).astype(jnp.float32)[None, None]
    s = jnp.where(valid[None, None], s + bias[None], -jnp.inf)
    m = jnp.max(s, -1, keepdims=True)
    p = jnp.exp(s - m)
    l = jnp.sum(p, -1, keepdims=True)
    o = jnp.einsum('nhbqk,nhbkd->nhbqd', (p / l).astype(v.dtype), vband)
    lse = (m + jnp.log(l))[..., 0]
    o = o.reshape(N, H, nb * CHUNK, Dh)[:, :, :L]
    lse = lse.reshape(N, H, nb * CHUNK)[:, :, :L]
    o = o.reshape(B, dilation, H, L, Dh).transpose(0, 3, 1, 2, 4).reshape(B, S, H, Dh)
    lse = lse.reshape(B, dilation, H, L).transpose(0, 3, 1, 2).reshape(B, S, H)
    return o, lse


def dilated_mixture(q, k, v, slopes):
    outs, lses = [], []
    for g, (window, dilation) in enumerate(DIL_PAIRS):
        hs = slice(g * HEADS_PER_DIL, (g + 1) * HEADS_PER_DIL)
        o, lse = dilated_branch(q[:, :, hs], k[:, :, hs], v[:, :, hs], window, dilation, slopes[hs])
        outs.append(o)
        lses.append(lse)
    O = jnp.stack(outs, 0)
    wts = jax.nn.softmax(jnp.stack(lses, 0), 0)
    return jnp.sum(wts[..., None].astype(O.dtype) * O, 0)


def memory_attention(q, mk, mv):
    s = jnp.einsum('bshd,bmhd->bhsm', q, mk).astype(jnp.float32) * ATT_SCALE
    p = jax.nn.softmax(s, -1).astype(mv.dtype)
    return jnp.einsum('bhsm,bmhd->bshd', p, mv)


def route(h, w_router, b_router):
    T = h.shape[0]
    probs = jax.nn.softmax((h @ w_router).astype(jnp.float32), -1)
    sel = probs + b_router.astype(jnp.float32)
    grp = sel.reshape(T, N_EXPERT_GROUPS, EXPERTS_PER_GROUP)
    grp_score = jnp.sum(lax.top_k(grp, TOP_K)[0], -1)
    g_idx = jnp.argmax(grp_score, -1).astype(jnp.int32)
    in_grp = jnp.take_along_axis(grp, g_idx[:, None, None], axis=1)[:, 0]
    _, local = lax.top_k(in_grp, TOP_K)
    expert = g_idx[:, None] * EXPERTS_PER_GROUP + local.astype(jnp.int32)
    gate = jnp.take_along_axis(probs, expert, 1)
    gate = gate / jnp.sum(gate, -1, keepdims=True)
    return expert, gate


def moe_ffn(h, expert, gate, w_gate, w_up, w_down):
    T, D = h.shape
    n_slots = T * TOP_K
    e_flat = expert.reshape(-1)
    g_flat = gate.reshape(-1)
    tok = jnp.arange(n_slots, dtype=jnp.int32) // TOP_K
    order = jnp.argsort(e_flat)
    e_s, tok_s, g_s = e_flat[order], tok[order], g_flat[order]
    counts = jnp.bincount(e_flat, length=N_EXPERTS)
    starts = jnp.cumsum(counts) - counts
    padded = (counts + MOE_BLOCK - 1) // MOE_BLOCK * MOE_BLOCK
    pad_ends = jnp.cumsum(padded)
    pad_starts = pad_ends - padded
    dest = pad_starts[e_s] + (jnp.arange(n_slots) - starts[e_s])
    n_pad = n_slots + N_EXPERTS * MOE_BLOCK
    n_blocks = n_pad // MOE_BLOCK
    slot_tok = jnp.zeros((n_pad,), jnp.int32).at[dest].set(tok_s)
    slot_gate = jnp.zeros((n_pad,), jnp.float32).at[dest].set(g_s)
    block_start = jnp.arange(n_blocks) * MOE_BLOCK
    block_expert = jnp.minimum(jnp.searchsorted(pad_ends, block_start, side='right'), N_EXPERTS - 1)
    xb = h[slot_tok].reshape(n_blocks, MOE_BLOCK, D)

    def expert_block(args):
        xblk, e = args
        return (jax.nn.silu(xblk @ w_gate[e]) * (xblk @ w_up[e])) @ w_down[e]

    yb = lax.map(expert_block, (xb, block_expert))
    y = yb.reshape(n_pad, D) * slot_gate[:, None].astype(h.dtype)
    return jnp.zeros_like(h).at[slot_tok].add(y)


def _normal(k, shape, scale):
    return jax.random.normal(k, shape, jnp.float32) * scale


def setup_inputs(seed: int = 0) -> dict:
    key = jax.random.key(seed)
    ks = jax.random.split(key, 24)
    D = D_MODEL
    return {
        "x": _normal(ks[0], (BATCH, SEQ, D), 1.0),
        "mem": _normal(ks[1], (BATCH, N_MEM, D), 1.0),
        "w_in_a": _normal(ks[2], (N_A, D, 2 * SG_WIDTH + MEM_WIDTH), D ** -0.5),
        "w_out_a": _normal(ks[3], (N_A, SG_WIDTH + MEM_WIDTH, D), (SG_WIDTH + MEM_WIDTH) ** -0.5 * BETA),
        "sg_ln_g": 1.0 + _normal(ks[4], (N_A, SG_WIDTH), 0.02),
        "sg_ln_b": _normal(ks[5], (N_A, SG_WIDTH), 0.02),
        "sg_w": _normal(ks[6], (N_A, N_SG, CHUNK, CHUNK), 0.5 * CHUNK ** -0.5),
        "sg_b": 1.0 + _normal(ks[7], (N_A, N_SG, CHUNK), 0.1),
        "w_in_b": _normal(ks[8], (N_B, D, DIL_Q_WIDTH + MEM_WIDTH), D ** -0.5),
        "w_out_b": _normal(ks[9], (N_B, DIL_OUT_WIDTH + MEM_WIDTH, D), (DIL_OUT_WIDTH + MEM_WIDTH) ** -0.5 * BETA),
        "w_k_shared": _normal(ks[10], (D, DIL_Q_WIDTH), D ** -0.5),
        "w_v_shared": _normal(ks[11], (D, DIL_Q_WIDTH), D ** -0.5 * BETA),
        "w_mem_k": _normal(ks[12], (DEPTH, D, MEM_WIDTH), D ** -0.5),
        "w_mem_v": _normal(ks[13], (DEPTH, D, MEM_WIDTH), D ** -0.5 * BETA),
        "ln1_g": 1.0 + _normal(ks[14], (DEPTH, D), 0.02),
        "ln1_b": _normal(ks[15], (DEPTH, D), 0.02),
        "ln2_g": 1.0 + _normal(ks[16], (DEPTH, D), 0.02),
        "ln2_b": _normal(ks[17], (DEPTH, D), 0.02),
        "w_router": _normal(ks[18], (D, N_EXPERTS), D ** -0.5),
        "b_router": _normal(ks[19], (N_EXPERTS,), 0.01),
        "w_gate": _normal(ks[20], (DEPTH, N_EXPERTS, D, D_EXPERT), D ** -0.5),
        "w_up": _normal(ks[21], (DEPTH, N_EXPERTS, D, D_EXPERT), D ** -0.5),
        "w_down": _normal(ks[22], (DEPTH, N_EXPERTS, D_EXPERT, D), D_EXPERT ** -0.5 * BETA),
    }


def reference(x, mem, w_in_a, w_out_a, sg_ln_g, sg_ln_b, sg_w, sg_b, w_in_b, w_out_b,
              w_k_shared, w_v_shared, w_mem_k, w_mem_v, ln1_g, ln1_b, ln2_g, ln2_b,
              w_router, b_router, w_gate, w_up, w_down):
    B, S, D = x.shape
    M = mem.shape[1]
    slopes = alibi_slopes()
    k_sh = None
    v_sh = None
    for layer in range(DEPTH):
        mk = (mem @ w_mem_k[layer]).reshape(B, M, MEM_HEADS, HEAD_DIM)
        mv = (mem @ w_mem_v[layer]).reshape(B, M, MEM_HEADS, HEAD_DIM)
        if layer < N_A:
            a = layer
            proj = x @ w_in_a[a]
            u = jax.nn.gelu(proj[..., :SG_WIDTH], approximate=False)
            gv = jax.nn.gelu(proj[..., SG_WIDTH:2 * SG_WIDTH], approximate=False)
            qm = proj[..., 2 * SG_WIDTH:].reshape(B, S, MEM_HEADS, HEAD_DIM)
            gv = layer_norm(gv, sg_ln_g[a], sg_ln_b[a])
            mix = spatial_gating(u.reshape(B, S, N_SG, HEAD_DIM), gv.reshape(B, S, N_SG, HEAD_DIM),
                                 sg_w[a], sg_b[a]).reshape(B, S, SG_WIDTH)
            mo = memory_attention(qm, mk, mv).reshape(B, S, MEM_WIDTH)
            att = jnp.concatenate([mix, mo], -1) @ w_out_a[a]
        else:
            bl = layer - N_A
            if bl == 0:
                k_sh = (x @ w_k_shared).reshape(B, S, N_DIL_HEADS, HEAD_DIM)
                v_sh = (x @ w_v_shared).reshape(B, S, N_DIL_HEADS, HEAD_DIM)
            proj = x @ w_in_b[bl]
            q = proj[..., :DIL_Q_WIDTH].reshape(B, S, N_DIL_HEADS, HEAD_DIM)
            qm = proj[..., DIL_Q_WIDTH:].reshape(B, S, MEM_HEADS, HEAD_DIM)
            mix = dilated_mixture(q, k_sh, v_sh, slopes).reshape(B, S, DIL_OUT_WIDTH)
            mo = memory_attention(qm, mk, mv).reshape(B, S, MEM_WIDTH)
            att = jnp.concatenate([mix, mo], -1) @ w_out_b[bl]
        x = layer_norm(ALPHA * x + att, ln1_g[layer], ln1_b[layer])
        h = x.reshape(B * S, D)
        expert, gate = route(h, w_router, b_router)
        ffn = moe_ffn(h, expert, gate, w_gate[layer], w_up[layer], w_down[layer]).reshape(B, S, D)
        x = layer_norm(ALPHA * x + ffn, ln2_g[layer], ln2_b[layer])
    return x
```

```python
import math
import numpy as np
import concourse.bass as bass
import concourse.mybir as mybir
from concourse.bass_utils import run_bass_kernel_spmd

F32 = mybir.dt.float32
BF16 = mybir.dt.bfloat16
U8 = mybir.dt.uint8
AF = mybir.ActivationFunctionType
ALU = mybir.AluOpType
AX = mybir.AxisListType

S = 2048
D = 1024
NT = 16
ALPHA = 4.0 ** 0.25
LN_EPS = 1e-5
SB_BASE = 16640
CAP = 229376 - SB_BASE - 64
PS_ROW = 16384
BLK = 32
ENG = ["pe", "act", "dve", "pool"]
NEG = -10000.0


def _esz(dt):
    if dt == F32:
        return 4
    if dt == BF16:
        return 2
    return 1


class Sched:
    def __init__(self, nc):
        self.nc = nc
        self.h = {"pe": nc.tensor, "act": nc.scalar, "dve": nc.vector, "pool": nc.gpsimd, "sp": nc.sync}
        self.sem = {e: nc.alloc_semaphore("s_" + e) for e in ENG}
        self.cnt = {e: 0 for e in ENG}
        self.NDS = 24
        self.dsem = [nc.alloc_semaphore("d%d" % i) for i in range(self.NDS)]
        self.ndma = 0
        nb = {"sb": CAP // BLK, "ps": PS_ROW // BLK}
        self.W = {k: np.zeros((4, 4, n), np.int64) for k, n in nb.items()}
        self.R = {k: np.zeros((4, 4, n), np.int64) for k, n in nb.items()}
        self.Wd = {k: np.zeros((4, n), np.int64) for k, n in nb.items()}
        self.Rd = {k: np.zeros((4, n), np.int64) for k, n in nb.items()}
        self.known = {w: {e: 0 for e in ENG} for w in ENG + ["sp"]}
        self.kd = {w: [0] * self.NDS for w in ENG + ["sp"]}

    @staticmethod
    def region(ap):
        name = ap.tensor.name
        if name.startswith("big"):
            sp, row = "sb", CAP
        elif name.startswith("psum"):
            sp, row = "ps", PS_ROW
        else:
            return None
        e = _esz(ap.dtype)
        pairs = ap.ap
        off = ap.offset * e
        p0 = off // row
        b0 = off % row
        npart = pairs[0][1]
        ext = e
        for st, c in pairs[1:]:
            ext += (c - 1) * abs(st) * e
        assert b0 + ext <= row, (name, b0, ext)
        return (sp, p0 // 32, (p0 + npart + 31) // 32, b0 // BLK, (b0 + ext + BLK - 1) // BLK)

    def _wait_eng(self, w, e, v):
        if v > self.known[w][e]:
            self.h[w].wait_ge(self.sem[e], int(v))
            self.known[w][e] = int(v)

    def _wait_dma(self, w, d):
        if d <= 0:
            return
        s = (d - 1) % self.NDS
        v = 16 * ((d - 1) // self.NDS + 1)
        if v > self.kd[w][s]:
            self.h[w].wait_ge(self.dsem[s], v)
            self.kd[w][s] = v

    def sync(self, eng, rr, ww):
        need = {e: 0 for e in ENG}
        dmas = set()
        for (sp, q0, q1, k0, k1) in rr:
            Wv = self.W[sp][:, q0:q1, k0:k1]
            for i, e in enumerate(ENG):
                need[e] = max(need[e], int(Wv[i].max()))
            dmas.update(np.unique(self.Wd[sp][q0:q1, k0:k1]).tolist())
        for (sp, q0, q1, k0, k1) in ww:
            Wv = self.W[sp][:, q0:q1, k0:k1]
            Rv = self.R[sp][:, q0:q1, k0:k1]
            for i, e in enumerate(ENG):
                if e == eng:
                    continue
                need[e] = max(need[e], int(Wv[i].max()), int(Rv[i].max()))
            dmas.update(np.unique(self.Wd[sp][q0:q1, k0:k1]).tolist())
            dmas.update(np.unique(self.Rd[sp][q0:q1, k0:k1]).tolist())
        if eng == "pe":
            need["pe"] = 0
        for e in ENG:
            if need[e] > 0:
                self._wait_eng(eng, e, need[e])
        for d in sorted(dmas):
            self._wait_dma(eng, int(d))

    def op(self, eng, fn, reads, writes, inc=True):
        rr = [r for r in (self.region(a) for a in reads) if r is not None]
        ww = [r for r in (self.region(a) for a in writes) if r is not None]
        self.sync(eng, rr, ww)
        ins = fn()
        if inc:
            self.cnt[eng] += 1
            ins.then_inc(self.sem[eng], 1)
            t = self.cnt[eng]
        else:
            t = self.cnt[eng] + 1
        i = ENG.index(eng)
        for (sp, q0, q1, k0, k1) in rr:
            self.R[sp][i, q0:q1, k0:k1] = t
        for (sp, q0, q1, k0, k1) in ww:
            self.W[sp][i, q0:q1, k0:k1] = t

    def dma(self, out, in_, queue="sp"):
        rr = [r for r in (self.region(in_),) if r is not None]
        ww = [r for r in (self.region(out),) if r is not None]
        self.sync(queue, rr, ww)
        self.ndma += 1
        d = self.ndma
        if d > self.NDS:
            self._wait_dma(queue, d - self.NDS)
        s = (d - 1) % self.NDS
        self.h[queue].dma_start(out=out, in_=in_).then_inc(self.dsem[s], 16)
        for (sp, q0, q1, k0, k1) in rr:
            self.Rd[sp][q0:q1, k0:k1] = d
        for (sp, q0, q1, k0, k1) in ww:
            self.Wd[sp][q0:q1, k0:k1] = d

    def finish(self):
        for d in range(max(1, self.ndma - self.NDS + 1), self.ndma + 1):
            self._wait_dma("sp", d)


class Builder:
    def __init__(self, nc, dbg=None):
        self.nc = nc
        self.dbg = dbg
        self.sc = Sched(nc)
        self.big = nc.alloc_sbuf_tensor_at("big", [128, CAP], U8, offset=SB_BASE)
        self.psum = nc.alloc_psum_tensor("psum", [128, 4096], F32)
        self.stg_i = 0
        self.ev_i = 0
        self.cast_eng = "dve"
        self.deferred = []
        self.l1_pre = False

    def sb(self, off, shape, dt, parts=128, p0=0):
        e = _esz(dt)
        n = int(np.prod(shape))
        assert off % 4 == 0 and off + n * e <= CAP, (off, shape)
        v = self.big[p0:p0 + parts, off:off + n * e].bitcast(dt)
        if len(shape) == 2:
            v = v.rearrange("p (a b) -> p a b", b=shape[1])
        elif len(shape) == 3:
            v = v.rearrange("p (a b c) -> p a b c", b=shape[1], c=shape[2])
        return v

    def bank(self, i, parts=128):
        return self.psum[0:parts, 512 * i:512 * (i + 1)]

    def bankbf(self, i):
        return self.psum[:, 512 * i:512 * (i + 1)].bitcast(BF16)[:, 0:512]

    def mm(self, out, lhsT, rhs, start, stop, inc=None):
        if inc is None:
            inc = stop
        nc = self.nc
        self.sc.op("pe", lambda: nc.tensor.matmul(out, lhsT, rhs, start=start, stop=stop, skip_group_check=True),
                   [lhsT, rhs], [out], inc=inc)

    def tr(self, out, in_, ident, inc):
        nc = self.nc
        self.sc.op("pe", lambda: nc.tensor.transpose(out, in_, ident), [in_, ident], [out], inc=inc)

    def act(self, out, in_, func, scale=1.0, bias=0.0, eng="act"):
        nc = self.nc
        reads = [in_]
        if not isinstance(scale, float):
            reads.append(scale)
        if not isinstance(bias, float):
            reads.append(bias)
        self.sc.op("act", lambda: nc.scalar.activation(out=out, in_=in_, func=func, bias=bias, scale=scale),
                   reads, [out])

    def copy(self, eng, out, in_):
        nc = self.nc
        if eng == "act":
            self.sc.op("act", lambda: nc.scalar.copy(out, in_), [in_], [out])
        elif eng == "dve":
            self.sc.op("dve", lambda: nc.vector.tensor_copy(out, in_), [in_], [out])
        else:
            self.sc.op("pool", lambda: nc.gpsimd.tensor_copy(out, in_), [in_], [out])

    def tt(self, out, in0, in1, op, eng="dve"):
        h = self.sc.h[eng]
        self.sc.op(eng, lambda: h.tensor_tensor(out, in0, in1, op), [in0, in1], [out])

    def ts(self, out, in0, s1, s2, op0, op1=None, eng="dve"):
        h = self.sc.h[eng]
        reads = [in0] + [s for s in (s1, s2) if s is not None and not isinstance(s, float)]
        if op1 is None:
            self.sc.op(eng, lambda: h.tensor_scalar(out, in0, s1, None, op0), reads, [out])
        else:
            self.sc.op(eng, lambda: h.tensor_scalar(out, in0, s1, s2, op0, op1), reads, [out])

    def stt(self, out, in0, scalar, in1, op0, op1, eng="dve"):
        h = self.sc.h[eng]
        reads = [in0, in1] + ([] if isinstance(scalar, float) else [scalar])
        self.sc.op(eng, lambda: h.scalar_tensor_tensor(out, in0, scalar, in1, op0, op1), reads, [out])

    def red(self, out, in_, op, eng="dve"):
        h = self.sc.h[eng]
        self.sc.op(eng, lambda: h.tensor_reduce(out, in_, AX.X, op), [in_], [out])

    def recip(self, out, in_):
        nc = self.nc
        self.sc.op("dve", lambda: nc.vector.reciprocal(out, in_), [in_], [out])

    def memset(self, eng, out, val):
        h = self.sc.h[eng]
        self.sc.op(eng, lambda: h.memset(out, val), [], [out])

    def dma(self, out, in_, queue="sp"):
        self.sc.dma(out, in_, queue)

    def load_w(self, dram, dst, pk=128, cast=None, maxn=1024):
        K, N = dram.shape
        self.dma(dst[0:pk, 0:K // pk, 0:N], dram.rearrange("(kc p) n -> p kc n", p=pk), queue="pool")

    def tok2feat(self, banks=(0, 1), scale=1.0, Ts=(0, 1, 2, 3)):
        for T in Ts:
            for kc in range(8):
                bk = self.bank(banks[self.ev_i % len(banks)])
                for j in range(4):
                    t = 4 * T + j
                    self.tr(bk[:, j * 128:(j + 1) * 128], self.XRES[:, t, kc * 128:(kc + 1) * 128], self.IDENT,
                            inc=(j == 3))
                eng = "act" if (self.ev_i % 2 == 0) else "dve"
                self.ev_i += 1
                dst = self.XT[:, kc, T * 512:(T + 1) * 512]
                if scale == 1.0:
                    self.copy(eng, dst, bk)
                elif eng == "act":
                    self.act(dst, bk, AF.Copy, scale=float(scale))
                else:
                    self.ts(dst, bk, float(scale), None, ALU.mult)

    def ln_stats(self, t, src_chunks):
        nc = self.nc
        st = self.BNST[:, t % 2, :]
        for c, src in enumerate(src_chunks):
            dst = st[:, c * 6:(c + 1) * 6]
            self.sc.op("dve", lambda s_=src, d_=dst: nc.vector.bn_stats(d_, s_), [src], [dst])
        mv = self.MVALL[:, t, :]
        self.sc.op("dve", lambda: nc.vector.bn_aggr(mv, st), [st], [mv])

    def xres_stats(self, t):
        self.ln_stats(t, [self.XRES[:, t, c * 512:(c + 1) * 512] for c in range(2)])

    def ln_rstd(self):
        self.ts(self.VE, self.MVALL[:, :, 1], LN_EPS, None, ALU.add)
        self.act(self.VE, self.VE, AF.Sqrt)
        self.recip(self.RSTD, self.VE)

    def ln_load(self, g_dram, b_dram, oscale=1.0):
        self.dma(self.LNG, g_dram.partition_broadcast(128))
        self.dma(self.LNB, b_dram.partition_broadcast(128))
        if oscale != 1.0:
            self.ts(self.LNG, self.LNG, float(oscale), None, ALU.mult)
            self.ts(self.LNB, self.LNB, float(oscale), None, ALU.mult)

    def ln_block(self, g_dram, b_dram, oscale, make_xt, gF=None, bF=None, stats_done=False):
        self.ln_load(g_dram, b_dram, oscale)
        if make_xt:
            self.dma(self.GF, gF)
            self.dma(self.BF, bF)
        if not stats_done:
            for t in range(NT):
                self.xres_stats(t)
        self.ln_rstd()
        self.stt(self.NMR, self.MVALL[:, :, 0], -1.0, self.RSTD, ALU.mult, ALU.mult)

        def norm(T):
            for t in range(4 * T, 4 * T + 4):
                x = self.XRES[:, t, :]
                if t % 3 == 2 and make_xt:
                    self.ts(x, x, self.MVALL[:, t, 0:1], self.RSTD[:, t:t + 1], ALU.subtract, ALU.mult)
                else:
                    self.act(x, x, AF.Identity, scale=self.RSTD[:, t:t + 1], bias=self.NMR[:, t:t + 1])

        def affine(t):
            x = self.XRES[:, t, :]
            self.tt(x, x, self.LNG, ALU.mult)
            self.tt(x, x, self.LNB, ALU.add)

        def xpose(T):
            for kc in range(8):
                bk = self.bank(self.ev_i % 2)
                self.ev_i += 1
                for j in range(4):
                    self.tr(bk[:, j * 128:(j + 1) * 128], self.XRES[:, 4 * T + j, kc * 128:(kc + 1) * 128], self.IDENT,
                            inc=(j == 3))
                if kc % 2 == 0:
                    self.ts(self.XT[:, kc, T * 512:(T + 1) * 512], bk, self.GF[:, kc:kc + 1], self.BF[:, kc:kc + 1],
                            ALU.mult, ALU.add)
                else:
                    self.act(self.XT[:, kc, T * 512:(T + 1) * 512], bk, AF.Identity, scale=self.GF[:, kc:kc + 1],
                             bias=self.BF[:, kc:kc + 1])
        if not make_xt:
            for T in range(4):
                norm(T)
                for t in range(4 * T, 4 * T + 4):
                    affine(t)
            return
        norm(0)
        for T in range(4):
            if T + 1 < 4:
                norm(T + 1)
            xpose(T)
        for t in range(NT):
            self.deferred.append(lambda t=t: affine(t))

    def mem_kv(self, memT, wk, wv, wkv2=None):
        self.load_w(memT, self.MEMT)
        self.load_w(wk, self.WKV, maxn=256)
        wv_buf = wkv2 if wkv2 is not None else self.WKV
        if wkv2 is not None:
            self.load_w(wv, wv_buf, maxn=256)
        for hp in range(2):
            bk = self.bank(hp)
            for kc in range(8):
                self.mm(bk[:, 0:256], self.WKV[:, kc, hp * 128:(hp + 1) * 128], self.MEMT[:, kc, :], kc == 0, kc == 7)
            self.copy("dve", self.MKT[:, hp, :], bk[:, 0:256])
        if wkv2 is None:
            self.load_w(wv, wv_buf, maxn=256)
        self.memset("dve", self.MV[:, :, :, 64:65], 1.0)
        for mc in range(2):
            bk = self.bank(2 + mc)
            for kc in range(8):
                self.mm(bk[:, 0:256], self.MEMT[:, kc, mc * 128:(mc + 1) * 128], wv_buf[:, kc, :], kc == 0, kc == 7)
            self.copy("dve", self.MV[:, mc, :, 0:64], bk[:, 0:256].rearrange("p (h d) -> p h d", d=64))

    def mem_attention(self):
        units = [(h, T) for h in range(4) for T in range(4)]

        def stage1(it):
            h, T = units[it]
            hp, po = h // 2, (h % 2) * 64
            sb0 = 0 + 2 * (it % 2)
            pt = self.PT[it % 2]
            for mc in range(2):
                self.mm(self.bank(sb0 + mc), self.MKT[po:po + 64, hp, mc * 128:(mc + 1) * 128],
                        self.QMT[po:po + 64, hp, T * 512:(T + 1) * 512], True, True)
                self.act(pt[:, mc, :], self.bank(sb0 + mc), AF.Exp, scale=0.125)

        def stage2(it):
            h, T = units[it]
            ob = 4 + 2 * (it % 2)
            pt = self.PT[it % 2]
            bo = self.bank(ob, 65)
            bd = self.bank(ob + 1, 64)
            for mc in range(2):
                self.mm(bo, self.MV[:, mc, h, :], pt[:, mc, :], mc == 0, mc == 1)
            self.finish_softmax(bo, bd, self.REC[it % 2], self.MOT[0:64, h, T * 512:(T + 1) * 512])
        for it in range(len(units) + 1):
            if it < len(units):
                stage1(it)
            if it >= 1:
                stage2(it - 1)

    def finish_softmax(self, bo, bd, rc, dst):
        self.copy("act", self.DROW, bo[64:65, :])
        self.mm(bd, self.ONESF[64:65, :], self.DROW, True, True)
        self.recip(rc, bd)
        self.tt(dst, bo[0:64, :], rc, ALU.mult)

    def out_proj_res(self, chunks, prescaled):
        for t in range(NT):
            for nh in range(2):
                bk = self.bank((2 * t + nh) % 4)
                n = len(chunks)
                for i, (lf, wf) in enumerate(chunks):
                    self.mm(bk, lf(t), wf(nh), i == 0, i == n - 1)
                x = self.XRES[:, t, nh * 512:(nh + 1) * 512]
                if prescaled:
                    self.tt(x, bk, x, ALU.add)
                else:
                    self.stt(x, x, ALPHA, bk, ALU.mult, ALU.add)
            self.xres_stats(t)

    def routing_load(self, wr_dram, br_dram):
        self.load_w(wr_dram, self.WRT, maxn=16, cast="dve")
        self.dma(self.BRT, br_dram.partition_broadcast(128))

    def routing(self):
        nc = self.nc
        bk = self.bank(7)
        for t in range(NT):
            for kc in range(8):
                self.mm(bk[:, t * 16:(t + 1) * 16], self.XT[:, kc, t * 128:(t + 1) * 128], self.WRT[:, kc, :],
                        kc == 0, kc == 7)
        RT = self.RT
        L3 = bk[:, 0:256].rearrange("p (t e) -> p t e", e=16)

        def v3(i):
            return RT[:, i, :].rearrange("p (t e) -> p t e", e=16)

        def v4(i):
            return RT[:, i, :].rearrange("p (a b) -> p a b", b=4)
        mx = self.RS[:, 0, :]
        self.red(mx, L3, ALU.max)
        self.tt(v3(0), L3, mx.unsqueeze(2).to_broadcast([128, 16, 16]), ALU.subtract)
        self.act(RT[:, 1, :], RT[:, 0, :], AF.Exp)
        sm = self.RS[:, 1, :]
        self.red(sm, v3(1), ALU.add)
        rs = self.RS[:, 2, :]
        self.recip(rs, sm)
        self.tt(v3(2), v3(1), rs.unsqueeze(2).to_broadcast([128, 16, 16]), ALU.mult)
        self.tt(v3(3), v3(2), self.BRT.unsqueeze(1).to_broadcast([128, 16, 16]), ALU.add)
        S4 = v4(3)
        PR = self.RT6
        self.tt(PR[:, :, 0:3], S4[:, :, 0:3], S4[:, :, 1:4], ALU.add)
        self.tt(PR[:, :, 3:5], S4[:, :, 0:2], S4[:, :, 2:4], ALU.add)
        self.tt(PR[:, :, 5:6], S4[:, :, 0:1], S4[:, :, 3:4], ALU.add)
        GS = self.RS[:, 3:7, :].rearrange("p a b -> p (a b)")
        self.red(GS, PR, ALU.max)
        gmx = self.RS[:, 7, :]
        self.red(gmx, GS.rearrange("p (t g) -> p t g", g=4), ALU.max)
        GM = self.RS[:, 8:12, :].rearrange("p a b -> p (a b)").rearrange("p (t g) -> p t g", g=4)
        self.tt(GM, GS.rearrange("p (t g) -> p t g", g=4), gmx.unsqueeze(2).to_broadcast([128, 16, 4]), ALU.is_ge)
        for j in range(4):
            self.tt(v4(4 + j) if j < 3 else v4(7), S4[:, :, j:j + 1].to_broadcast([128, 64, 4]), S4, ALU.is_gt)
        self.tt(v4(4), v4(4), v4(5), ALU.add)
        self.tt(v4(6), v4(6), v4(7), ALU.add)
        self.tt(v4(4), v4(4), v4(6), ALU.add)
        self.ts(v4(5), v4(4), 1.5, None, ALU.is_lt)
        self.tt(v4(6), v4(5), GM.rearrange("p t g -> p (t g)").unsqueeze(2).to_broadcast([128, 64, 4]), ALU.mult)
        self.tt(RT[:, 7, :], RT[:, 2, :], RT[:, 6, :], ALU.mult)
        g2 = self.RS[:, 12, :]
        self.red(g2, v3(7), ALU.add)
        rg = self.RS[:, 13, :]
        self.recip(rg, g2)
        self.tt(self.GATES, v3(7), rg.unsqueeze(2).to_broadcast([128, 16, 16]), ALU.mult)

    def moe_load(self, wg, wu, wd, e):
        ring = self.WRING
        for m, w in enumerate((wg, wu, wd)):
            self.load_w(w[e], ring[(3 * e + m + 1) % 4], cast="pool")

    def moe(self, wg, wu, wd, hook=None):
        ring = self.WRING
        gu_i = 0
        dn_i = 0
        for e in range(16):
            Wg = ring[(3 * e + 1) % 4]
            Wu = ring[(3 * e + 2) % 4]
            Wd = ring[(3 * e + 3) % 4]
            if e > 0:
                self.moe_load(wg, wu, wd, e)
            if e == 15 and hook is not None:
                hook()
            for T in range(4):
                for fc in range(8):
                    bg = self.bank(0 + 2 * (gu_i % 2))
                    bu = self.bank(1 + 2 * (gu_i % 2))
                    sg = self.SG[gu_i % 2]
                    gu_i += 1
                    for kc in range(8):
                        self.mm(bg, Wg[:, kc, fc * 128:(fc + 1) * 128], self.XT[:, kc, T * 512:(T + 1) * 512],
                                kc == 0, kc == 7)
                    for kc in range(8):
                        self.mm(bu, Wu[:, kc, fc * 128:(fc + 1) * 128], self.XT[:, kc, T * 512:(T + 1) * 512],
                                kc == 0, kc == 7)
                    self.act(sg, bg, AF.Silu)
                    self.tt(self.ACTT[:, fc, T * 512:(T + 1) * 512], sg, bu, ALU.mult)
                    self.drain(1)
            self.drain_all()
            for t in range(NT):
                b0 = 4 + 2 * (dn_i % 2)
                dn_i += 1
                for nh in range(2):
                    bk = self.bank(b0 + nh)
                    for fc in range(8):
                        self.mm(bk, self.ACTT[:, fc, t * 128:(t + 1) * 128], Wd[:, fc, nh * 512:(nh + 1) * 512],
                                fc == 0, fc == 7)
                y = self.XRES[:, t, :]
                pv = self.psum[:, 512 * b0:512 * (b0 + 2)]
                self.stt(y, pv, self.GATES[:, t, e:e + 1], y, ALU.mult, ALU.add)
                if e == 15:
                    self.xres_stats(t)

    def drain(self, n=1):
        for _ in range(n):
            if self.deferred:
                self.deferred.pop(0)()

    def drain_all(self):
        while self.deferred:
            self.deferred.pop(0)()

    def l1_prefetch(self):
        I = self.I
        self.MEMT = self.sb(self.STG_OFF[0], [8, 256], BF16)
        self.WKV = self.sb(self.STG_OFF[1], [8, 256], BF16)
        self.mem_kv(I["memT"], I["w_mem_k"][1], I["w_mem_v"][1])
        for i in range(2):
            self.load_w(I["w_in_b"][:, i * 256:(i + 1) * 256], self.sb(self.STG_OFF[i], [8, 256], BF16), maxn=256)
        self.l1_pre = True

    def dump(self, dbg_out):
        self.drain_all()
        for t in range(NT):
            self.dma(dbg_out[t * 128:(t + 1) * 128, :], self.XRES[:, t, :], queue=("sp" if t % 2 == 0 else "act"))

    def build(self, I, out):
        nc = self.nc
        self.I = I
        o = 0

        def take(n):
            nonlocal o
            r = o
            o += (n + 63) // 64 * 64
            assert o <= CAP, o
            return r
        self.XRES = self.sb(take(65536), [16, 1024], F32)
        XT_OFF = take(32768)
        self.XT = self.sb(XT_OFF, [8, 2048], BF16)
        self.IDENT = self.sb(take(512), [128], F32)
        self.IDB = self.sb(take(256), [128], BF16)
        STG0_OFF = take(4096)
        STG1_OFF = take(4096)
        self.STG = [self.sb(STG0_OFF, [1024], F32), self.sb(STG1_OFF, [1024], F32)]
        self.STG_OFF = [STG0_OFF, STG1_OFF]
        self.GATES = self.sb(take(1024), [16, 16], F32)
        self.DROW = self.big[64:65, STG1_OFF:STG1_OFF + 2048].bitcast(F32)
        self.MVALL = self.sb(take(128), [16, 2], F32)
        self.VE = self.sb(take(64), [16], F32)
        self.RSTD = self.sb(take(64), [16], F32)
        self.NMR = self.sb(take(64), [16], F32)
        self.GF = self.sb(take(32), [8], F32)
        self.BF = self.sb(take(32), [8], F32)
        self.ONESF = self.sb(take(256), [64], F32)
        self.BNST = self.sb(take(96), [2, 12], F32)
        self.ONES = self.sb(take(128), [64], BF16)
        self.MKT = self.sb(take(1024), [2, 256], BF16)
        self.MV = self.sb(take(1040), [2, 4, 65], BF16)
        LN_OFF = take(4096)
        self.LNG = self.sb(LN_OFF, [1024], F32)
        self.LNB = self.sb(take(4096), [1024], F32)
        PH = o
        PHSZ = CAP - PH

        self.dma(self.IDENT, I["ident"])
        self.copy("dve", self.IDB, self.IDENT)
        self.memset("dve", self.ONES, 1.0)
        self.memset("dve", self.ONESF, 1.0)
        for t in range(0, NT, 4):
            self.dma(self.XRES[:, t:t + 4, :], I["x"][t * 128:(t + 4) * 128, :].rearrange("(a p) d -> p a d", p=128))

        A0 = PH
        B0 = PH + 28672
        C0 = PH + 53248
        D0 = PH + 77824
        E0 = PH + 86016
        WIN = self.sb(A0, [8, 1792], BF16)
        MIXT = self.sb(A0, [6, 2048], BF16)
        U = self.sb(B0, [16, 768], BF16)
        WOA = self.sb(B0, [6, 1024], BF16)
        WOM = self.sb(B0 + 12288, [4, 1024], BF16)
        GV = self.sb(C0, [16, 768], BF16)
        self.MOT = self.sb(C0, [4, 2048], BF16)
        self.PT = [self.sb(C0 + 16384 + i * 2048, [2, 512], BF16) for i in range(2)]
        self.REC = [self.sb(C0 + 20480 + i * 2048, [512], F32, parts=64) for i in range(2)]
        self.QMT = self.sb(D0, [2, 2048], BF16)
        SGW = self.sb(E0, [12, 128], BF16)
        BST = self.sb(E0 + 3072, [12], F32)
        TRIL = self.sb(E0 + 3072 + 256, [128], F32)
        self.MEMT = self.sb(B0, [8, 256], BF16)
        self.WKV = self.sb(B0 + 4096, [8, 256], BF16)
        assert E0 + 3072 + 256 + 512 <= CAP

        self.mem_kv(I["memT"], I["w_mem_k"][0], I["w_mem_v"][0], wkv2=self.sb(B0 + 8192, [8, 256], BF16))
        self.load_w(I["w_in_a"], WIN)
        self.dma(TRIL, I["tril"])
        self.dma(BST, I["sg_bT"])
        self.dma(self.LNG[:, 0:768], I["sg_ln_g"].partition_broadcast(128))
        self.dma(self.LNB[:, 0:768], I["sg_ln_b"].partition_broadcast(128))

        for T in range(4):
            self.tok2feat(Ts=(T,))
            for t in range(4 * T, 4 * T + 4):
                bs = [2, 3, 4] if t % 2 == 0 else [5, 6, 7]
                for nb in range(3):
                    for kc in range(8):
                        self.mm(self.bank(bs[nb]), self.XT[:, kc, t * 128:(t + 1) * 128],
                                WIN[:, kc, nb * 512:(nb + 1) * 512], kc == 0, kc == 7)
                gvt = self.STG[t % 2]
                self.act(U[:, t, 0:512], self.bank(bs[0]), AF.Gelu)
                self.act(U[:, t, 512:768], self.bank(bs[1])[:, 0:256], AF.Gelu)
                self.act(gvt[:, 0:256], self.bank(bs[1])[:, 256:512], AF.Gelu)
                self.act(gvt[:, 256:768], self.bank(bs[2]), AF.Gelu)
                self.ln_stats(t, [gvt[:, c * 384:(c + 1) * 384] for c in range(2)])
                self.copy("pool", GV[:, t, :], gvt[:, 0:768])
        for i in range(2):
            st = self.STG[self.stg_i % 2]
            self.stg_i += 1
            stv = st[:, 0:768].rearrange("p (g q) -> p g q", q=128)
            self.dma(st[:, 0:768], I["sg_wT"][:, i * 768:(i + 1) * 768])
            self.tt(SGW[:, 6 * i:6 * i + 6, :], stv, TRIL.unsqueeze(1).to_broadcast([128, 6, 128]), ALU.mult)
        i = 0
        for hp in range(2):
            for T in range(4):
                bk = self.bank(i % 2)
                i += 1
                for kc in range(8):
                    self.mm(bk, WIN[:, kc, 1536 + hp * 128:1536 + (hp + 1) * 128], self.XT[:, kc, T * 512:(T + 1) * 512],
                            kc == 0, kc == 7)
                self.copy("act", self.QMT[:, hp, T * 512:(T + 1) * 512], bk)
        self.ln_rstd()
        self.stt(self.NMR, self.MVALL[:, :, 0], -1.0, self.RSTD, ALU.mult, ALU.mult)
        for t in range(NT):
            gvt = self.STG[t % 2][:, 0:768]
            self.act(gvt, GV[:, t, :], AF.Identity, scale=self.RSTD[:, t:t + 1], bias=self.NMR[:, t:t + 1])
            self.tt(gvt, gvt, self.LNG[:, 0:768], ALU.mult)
            self.tt(GV[:, t, :], gvt, self.LNB[:, 0:768], ALU.add)
        i = 0
        for hf in range(2):
            for g in range(12):
                bk = self.bank(2 + i % 4)
                i += 1
                bk3 = bk.rearrange("p (c d) -> p c d", d=64)
                self.mm(bk3, SGW[:, g, :], GV[:, 8 * hf:8 * hf + 8, g * 64:(g + 1) * 64], True, True)
                uu = U[:, 8 * hf:8 * hf + 8, g * 64:(g + 1) * 64]
                ts_ = self.STG[i % 2][:, 0:512]
                self.act(ts_, bk, AF.Identity, bias=BST[:, g:g + 1])
                self.tt(uu, uu, ts_.rearrange("p (c d) -> p c d", d=64), ALU.mult)
        i = 0
        for fc in range(6):
            for T in range(4):
                b = i % 2
                i += 1
                bb = self.bankbf(b)
                for j in range(4):
                    self.tr(bb[:, j * 128:(j + 1) * 128], U[:, 4 * T + j, fc * 128:(fc + 1) * 128], self.IDB, inc=(j == 3))
                self.copy("act", MIXT[:, fc, T * 512:(T + 1) * 512], bb)
        self.load_w(I["w_out_a"][0:768, :], WOA)
        self.load_w(I["w_out_a"][768:1024, :], WOM, pk=64)
        self.mem_attention()
        chunks = [(lambda t, fc=fc: MIXT[:, fc, t * 128:(t + 1) * 128], lambda nh, fc=fc: WOA[:, fc, nh * 512:(nh + 1) * 512])
                  for fc in range(6)]
        chunks += [(lambda t, h=h: self.MOT[0:64, h, t * 128:(t + 1) * 128], lambda nh, h=h: WOM[0:64, h, nh * 512:(nh + 1) * 512])
                   for h in range(4)]
        self.out_proj_res(chunks, prescaled=False)
        if self.dbg == "r1_0":
            return self.dump(out)
        self.ffn_block(I, 0, LN_OFF)
        if self.dbg in ("ln1_0", "r2_0", "gates_0"):
            return self.dump(out)
        if self.dbg == "l0":
            return self.dump(out)

        QT = self.sb(PH, [6, 2048], BF16)
        KT = self.sb(PH + 24576, [6, 2048], BF16)
        V = self.sb(PH + 49152, [48, 4, 65], BF16)
        self.QMT = self.sb(PH + 74240, [2, 2048], BF16)
        W1O = PH + 82432
        W1 = [self.sb(self.STG_OFF[i], [8, 256], BF16) for i in range(2)]
        BASEA = self.sb(W1O + 8192, [128], F32)
        BASEB = self.sb(W1O + 8192 + 512, [128], F32)
        assert W1O + 8192 + 1024 <= CAP
        if not self.l1_pre:
            self.MEMT = W1[0]
            self.WKV = W1[1]
            self.mem_kv(I["memT"], I["w_mem_k"][1], I["w_mem_v"][1])
        self.dma(BASEA, I["baseA"])
        self.dma(BASEB, I["baseB"])
        wi = 0
        bi = 0
        for (wsrc, c0, dst, npair) in ((I["w_in_b"], 0, QT, 6), (I["w_k"], 0, KT, 6), (I["w_in_b"], 768, self.QMT, 2)):
            for pp in range(0, npair, 2):
                w = W1[wi % 2]
                if not (self.l1_pre and wi < 2):
                    self.load_w(wsrc[:, c0 + pp * 128:c0 + (pp + 2) * 128], w, maxn=256)
                wi += 1
                for p2 in range(2):
                    for T in range(4):
                        bk = self.bank(bi % 2)
                        bi += 1
                        for kc in range(8):
                            self.mm(bk, w[:, kc, p2 * 128:(p2 + 1) * 128], self.XT[:, kc, T * 512:(T + 1) * 512],
                                    kc == 0, kc == 7)
                        self.copy("act" if bi % 2 else "dve", dst[:, pp + p2, T * 512:(T + 1) * 512], bk)
                        if bi % 2 == 0:
                            self.drain(1)
        self.memset("dve", V[:, :, :, 64:65], 1.0)
        for g in range(3):
            w = W1[wi % 2]
            wi += 1
            self.load_w(I["w_v"][:, g * 256:(g + 1) * 256], w, maxn=256)
            for idx in range(16):
                if g == 0:
                    tok = slice(idx * 128, (idx + 1) * 128)
                elif g == 1:
                    t0 = (idx // 4) + 512 * (idx % 4)
                    tok = slice(t0, t0 + 4 * 127 + 1, 4)
                else:
                    tok = slice(idx, idx + 16 * 127 + 1, 16)
                bk = self.bank(2 + (bi % 2))
                bi += 1
                for kc in range(8):
                    self.mm(bk[:, 0:256], self.XT[:, kc, tok], w[:, kc, :], kc == 0, kc == 7)
                self.copy("act" if bi % 2 else "dve", V[:, g * 16 + idx, :, 0:64],
                          bk[:, 0:256].rearrange("p (h d) -> p h d", d=64))
                if bi % 4 == 0:
                    self.drain(1)
        self.drain_all()
        MIXB = self.sb(XT_OFF, [4, 2048], BF16)
        self.MOT = self.sb(XT_OFF + 16384, [4, 2048], BF16)
        NB = 4
        TMP = [self.sb(W1O + i * 2048, [512], F32) for i in range(NB)]
        PTD = [self.sb(STG0_OFF + i * 1024, [512], BF16) for i in range(NB)]
        RECD = self.sb(STG1_OFF + 2048, [512], F32, parts=64)
        slopes = [2.0 ** (-8.0 * (h + 1) / 12.0) for h in range(12)]
        dil = [1, 4, 16]

        def sl(start, n, step=1):
            return slice(start, start + step * (n - 1) + 1, step)
        units = []
        ni = 0
        for j in range(4):
            for Q in range(4):
                num = self.bank(4 + 2 * (ni % 2), 65)
                den = self.bank(5 + 2 * (ni % 2), 64)
                ni += 1
                batches = []
                for g in range(3):
                    h = 4 * g + j
                    hp, po = h // 2, (h % 2) * 64
                    c8 = 8.0 * slopes[h] * dil[g]
                    A, Bp = [], []
                    if g == 0:
                        for i4 in range(4):
                            qb = 4 * Q + i4
                            qtok = sl(qb * 128, 128)
                            A.append((KT[po:po + 64, hp, qtok], QT[po:po + 64, hp, qtok], i4 * 128, 128, qb,
                                      sl(i4 * 128, 128)))
                            if qb >= 1:
                                Bp.append((KT[po:po + 64, hp, sl((qb - 1) * 128, 128)], QT[po:po + 64, hp, qtok],
                                           i4 * 128, 128, qb - 1, sl(i4 * 128, 128)))
                        batches.append((BASEA, 128, c8, A))
                        batches.append((BASEB, 128, c8, Bp))
                    elif g == 1:
                        for r in range(4):
                            qtok = sl(512 * Q + r, 128, 4)
                            A.append((KT[po:po + 64, hp, qtok], QT[po:po + 64, hp, qtok], r * 128, 128,
                                      16 + r * 4 + Q, sl(r, 128, 4)))
                            if Q >= 1:
                                Bp.append((KT[po:po + 64, hp, sl(512 * (Q - 1) + r, 128, 4)], QT[po:po + 64, hp, qtok],
                                           r * 128, 128, 16 + r * 4 + Q - 1, sl(r, 128, 4)))
                        batches.append((BASEA, 128, c8, A))
                        if Bp:
                            batches.append((BASEB, 128, c8, Bp))
                    else:
                        for r in range(16):
                            A.append((KT[po:po + 64, hp, sl(r, 128, 16)], QT[po:po + 64, hp, sl(512 * Q + r, 32, 16)],
                                      r * 32, 32, 32 + r, sl(r, 32, 16)))
                        batches.append((BASEA[:, 32 * Q:32 * Q + 32], 32, c8, A[0:8]))
                        batches.append((BASEA[:, 32 * Q:32 * Q + 32], 32, c8, A[8:16]))
                total = sum(len(b_[3]) for b_ in batches)
                k_i = 0
                for bi_, (base, w_, c8, items) in enumerate(batches):
                    flags = []
                    for _ in items:
                        flags.append((k_i == 0, k_i == total - 1))
                        k_i += 1
                    units.append((j, Q, num, den, base, w_, c8, items, flags, bi_ == len(batches) - 1))

        def d_stage1(u, si):
            (j, Q, num, den, base, w_, c8, items, flags, lastb) = u
            sbk = self.bank(si % 4)
            tmp = TMP[si % NB]
            ptd = PTD[si % NB]
            c_lo = items[0][2]
            c_hi = items[-1][2] + items[-1][3]
            for ii, (lt, rt, c0, ncol, vb, oc) in enumerate(items):
                self.mm(sbk[:, c0:c0 + ncol], lt, rt, True, True, inc=(ii == len(items) - 1))
            nrep = (c_hi - c_lo) // w_
            self.stt(tmp[:, c_lo:c_hi].rearrange("p (a b) -> p a b", b=w_),
                     base.unsqueeze(1).to_broadcast([128, nrep, w_]), c8,
                     sbk[:, c_lo:c_hi].rearrange("p (a b) -> p a b", b=w_), ALU.mult, ALU.add)
            self.act(ptd[:, c_lo:c_hi], tmp[:, c_lo:c_hi], AF.Exp, scale=0.125)

        def d_stage2(u, si):
            (j, Q, num, den, base, w_, c8, items, flags, lastb) = u
            ptd = PTD[si % NB]
            n_it = len(items)
            for ii, ((lt, rt, c0, ncol, vb, oc), (first, last)) in enumerate(zip(items, flags)):
                self.mm(num[:, oc], V[:, vb, j, :], ptd[:, c0:c0 + ncol], first, last, inc=(ii == n_it - 1))
            if lastb:
                self.copy("act", self.DROW, num[64:65, :])
                pend_fin.append((num, den, j, Q))

        def d_finish():
            while pend_fin:
                num, den, j, Q = pend_fin.pop(0)
                self.mm(den, self.ONESF[64:65, :], self.DROW, True, True)
                self.recip(RECD, den)
                self.tt(MIXB[0:64, j, Q * 512:(Q + 1) * 512], num[0:64, :], RECD, ALU.mult)
        pend_fin = []
        LA = 3
        for si in range(len(units) + LA):
            if si < len(units):
                d_stage1(units[si], si)
            d_finish()
            if si >= LA:
                d_stage2(units[si - LA], si - LA)
        d_finish()
        WOB = self.sb(PH, [8, 1024], BF16)
        self.load_w(I["w_out_b"], WOB, pk=64)
        self.PT = [self.sb(PH + 24576 + i * 2048, [2, 512], BF16) for i in range(2)]
        self.REC = [self.sb(PH + 24576 + 4096 + i * 2048, [512], F32, parts=64) for i in range(2)]
        self.mem_attention()
        chunks = [(lambda t, jj=jj: MIXB[0:64, jj, t * 128:(t + 1) * 128], lambda nh, jj=jj: WOB[0:64, jj, nh * 512:(nh + 1) * 512])
                  for jj in range(4)]
        chunks += [(lambda t, h=h: self.MOT[0:64, h, t * 128:(t + 1) * 128], lambda nh, h=h: WOB[0:64, 4 + h, nh * 512:(nh + 1) * 512])
                   for h in range(4)]
        self.out_proj_res(chunks, prescaled=True)
        if self.dbg == "r1_1":
            return self.dump(out)
        self.ffn_block(I, 1, LN_OFF)
        self.dump(out)

    def ffn_block(self, I, l, LN_OFF):
        self.WRING = [self.sb(LN_OFF + i * 16384, [8, 1024], BF16) for i in range(4)]
        self.ACTT = self.sb(LN_OFF + 65536, [8, 2048], BF16)
        self.SG = [self.sb(LN_OFF + 98304 + i * 1024, [512], BF16) for i in range(2)]
        assert LN_OFF + 98304 + 2048 <= CAP
        A_OFF = LN_OFF + 65536
        self.RT = self.sb(A_OFF, [8, 256], F32)
        self.RT6 = self.sb(A_OFF + 8192, [64, 6], F32)
        self.RS = self.sb(A_OFF + 8192 + 1536, [14, 16], F32)
        self.WRT = self.sb(A_OFF + 12288, [8, 16], BF16)
        self.BRT = self.sb(A_OFF + 12288 + 256, [16], F32)
        wg, wu, wd = I["w_gate"][l], I["w_up"][l], I["w_down"][l]
        self.routing_load(I["w_router"], I["b_router"])
        if self.dbg is None or self.dbg in ("r2_%d" % l, "l%d" % l, "r1_1"):
            self.moe_load(wg, wu, wd, 0)
        self.ln_block(I["ln1_g"][l], I["ln1_b"][l], ALPHA, make_xt=True, gF=I["ln1_gF"][l], bF=I["ln1_bF"][l],
                      stats_done=True)
        if self.dbg == "ln1_%d" % l:
            return
        self.routing()
        if self.dbg == "gates_%d" % l:
            self.drain_all()
            for t in range(NT):
                self.copy("dve", self.XRES[:, t, 0:16], self.GATES[:, t, :])
            return
        self.moe(wg, wu, wd, hook=(self.l1_prefetch if (l == 0 and self.dbg is None) else None))
        if self.dbg == "r2_%d" % l:
            return
        if l == 0:
            self.ln_block(I["ln2_g"][l], I["ln2_b"][l], ALPHA, make_xt=True, gF=I["ln2_gF"][l], bF=I["ln2_bF"][l],
                          stats_done=True)
        else:
            self.ln_block(I["ln2_g"][l], I["ln2_b"][l], 1.0, make_xt=False, stats_done=True)


def _dram_inputs(nc):
    def t(name, shape):
        return nc.dram_tensor(name, list(shape), F32, kind="ExternalInput").ap()
    I = {}
    I["x"] = t("x", (S, D))
    I["memT"] = t("memT", (D, 256))
    I["w_in_a"] = t("w_in_a", (D, 1792))
    I["w_out_a"] = t("w_out_a", (1024, D))
    I["sg_ln_g"] = t("sg_ln_g", (768,))
    I["sg_ln_b"] = t("sg_ln_b", (768,))
    I["sg_wT"] = t("sg_wT", (128, 1536))
    I["sg_bT"] = t("sg_bT", (128, 12))
    I["w_in_b"] = t("w_in_b", (D, 1024))
    I["w_out_b"] = t("w_out_b", (512, D))
    I["w_k"] = t("w_k", (D, 768))
    I["w_v"] = t("w_v", (D, 768))
    I["w_mem_k"] = t("w_mem_k", (2, D, 256))
    I["w_mem_v"] = t("w_mem_v", (2, D, 256))
    for n in ("ln1_g", "ln1_b", "ln2_g", "ln2_b"):
        I[n] = t(n, (2, D))
        I[n + "F"] = t(n + "F", (2, 128, 8))
    I["w_router"] = t("w_router", (D, 16))
    I["b_router"] = t("b_router", (16,))
    for n in ("w_gate", "w_up", "w_down"):
        I[n] = t(n, (2, 16, D, D))
    I["ident"] = t("ident", (128, 128))
    I["tril"] = t("tril", (128, 128))
    I["baseA"] = t("baseA", (128, 128))
    I["baseB"] = t("baseB", (128, 128))
    return I


def build_program(dbg=None):
    nc = bass.Bass("TRN2", target_bir_lowering=False)
    I = _dram_inputs(nc)
    out = nc.dram_tensor("out", [S, D], F32, kind="ExternalOutput").ap()
    b = Builder(nc, dbg)
    b.build(I, out)
    b.sc.finish()
    return nc


def host_shared(inputs):
    f = lambda a: np.ascontiguousarray(np.asarray(a, dtype=np.float32))
    m = {}
    m["w_in_a"] = f(inputs["w_in_a"][0])
    m["w_out_a"] = f(inputs["w_out_a"][0])
    m["sg_ln_g"] = f(inputs["sg_ln_g"][0])
    m["sg_ln_b"] = f(inputs["sg_ln_b"][0])
    m["sg_wT"] = f(np.transpose(np.asarray(inputs["sg_w"][0]), (2, 0, 1)).reshape(128, 1536))
    m["sg_bT"] = f(np.asarray(inputs["sg_b"][0]).T)
    m["w_in_b"] = f(inputs["w_in_b"][0])
    m["w_out_b"] = f(inputs["w_out_b"][0])
    m["w_k"] = f(inputs["w_k_shared"])
    m["w_v"] = f(inputs["w_v_shared"])
    for n in ("w_mem_k", "w_mem_v", "ln1_g", "ln1_b", "ln2_g", "ln2_b", "w_router", "b_router",
              "w_gate", "w_up", "w_down"):
        m[n] = f(inputs[n])
    for n in ("ln1_g", "ln1_b", "ln2_g", "ln2_b"):
        m[n + "F"] = f(np.asarray(inputs[n]).reshape(2, 8, 128).transpose(0, 2, 1))
    k = np.arange(128)[:, None]
    q = np.arange(128)[None, :]
    m["ident"] = np.eye(128, dtype=np.float32)
    m["tril"] = (k <= q).astype(np.float32)
    relA = (q - k).astype(np.float32)
    m["baseA"] = np.where(q - k >= 0, -relA, NEG).astype(np.float32)
    relB = (128 + q - k).astype(np.float32)
    m["baseB"] = np.where(k >= q, -relB, NEG).astype(np.float32)
    return m


def host_inputs(inputs, b, shared=None):
    f = lambda a: np.ascontiguousarray(np.asarray(a, dtype=np.float32))
    m = dict(shared if shared is not None else host_shared(inputs))
    m["x"] = f(inputs["x"][b])
    m["memT"] = f(np.asarray(inputs["mem"][b]).T)
    return m


def kernel(**inputs):
    n = 8
    nc = build_program()
    shared = host_shared(inputs)
    in_maps = [host_inputs(inputs, b, shared) for b in range(n)]
    res = run_bass_kernel_spmd(nc, in_maps, core_ids=list(range(n)))
    return np.stack([np.asarray(r["out"], dtype=np.float32) for r in res.results], axis=0)
```

```python
import math
import numpy as np
import concourse.bass as bass
import concourse.mybir as mybir
from concourse.bass_utils import run_bass_kernel_spmd

F32 = mybir.dt.float32
BF16 = mybir.dt.bfloat16
U8 = mybir.dt.uint8
AF = mybir.ActivationFunctionType
ALU = mybir.AluOpType
AX = mybir.AxisListType

S = 2048
D = 1024
NT = 16
ALPHA = 4.0 ** 0.25
LN_EPS = 1e-5
SB_BASE = 16640
CAP = 229376 - SB_BASE - 64
PS_ROW = 16384
BLK = 32
ENG = ["pe", "act", "dve", "pool"]
NEG = -10000.0


def _esz(dt):
    if dt == F32:
        return 4
    if dt == BF16:
        return 2
    return 1


class Sched:
    def __init__(self, nc):
        self.nc = nc
        self.h = {"pe": nc.tensor, "act": nc.scalar, "dve": nc.vector, "pool": nc.gpsimd, "sp": nc.sync}
        self.sem = {e: nc.alloc_semaphore("s_" + e) for e in ENG}
        self.cnt = {e: 0 for e in ENG}
        self.NDS = 24
        self.dsem = [nc.alloc_semaphore("d%d" % i) for i in range(self.NDS)]
        self.ndma = 0
        nb = {"sb": CAP // BLK, "ps": PS_ROW // BLK}
        self.W = {k: np.zeros((4, 4, n), np.int64) for k, n in nb.items()}
        self.R = {k: np.zeros((4, 4, n), np.int64) for k, n in nb.items()}
        self.Wd = {k: np.zeros((4, n), np.int64) for k, n in nb.items()}
        self.Rd = {k: np.zeros((4, n), np.int64) for k, n in nb.items()}
        self.known = {w: {e: 0 for e in ENG} for w in ENG + ["sp"]}
        self.kd = {w: [0] * self.NDS for w in ENG + ["sp"]}

    @staticmethod
    def region(ap):
        name = ap.tensor.name
        if name.startswith("big"):
            sp, row = "sb", CAP
        elif name.startswith("psum"):
            sp, row = "ps", PS_ROW
        else:
            return None
        e = _esz(ap.dtype)
        pairs = ap.ap
        off = ap.offset * e
        p0 = off // row
        b0 = off % row
        npart = pairs[0][1]
        ext = e
        for st, c in pairs[1:]:
            ext += (c - 1) * abs(st) * e
        assert b0 + ext <= row, (name, b0, ext)
        return (sp, p0 // 32, (p0 + npart + 31) // 32, b0 // BLK, (b0 + ext + BLK - 1) // BLK)

    def _wait_eng(self, w, e, v):
        if v > self.known[w][e]:
            self.h[w].wait_ge(self.sem[e], int(v))
            self.known[w][e] = int(v)

    def _wait_dma(self, w, d):
        if d <= 0:
            return
        s = (d - 1) % self.NDS
        v = 16 * ((d - 1) // self.NDS + 1)
        if v > self.kd[w][s]:
            self.h[w].wait_ge(self.dsem[s], v)
            self.kd[w][s] = v

    def sync(self, eng, rr, ww):
        need = {e: 0 for e in ENG}
        dmas = set()
        for (sp, q0, q1, k0, k1) in rr:
            Wv = self.W[sp][:, q0:q1, k0:k1]
            for i, e in enumerate(ENG):
                need[e] = max(need[e], int(Wv[i].max()))
            dmas.update(np.unique(self.Wd[sp][q0:q1, k0:k1]).tolist())
        for (sp, q0, q1, k0, k1) in ww:
            Wv = self.W[sp][:, q0:q1, k0:k1]
            Rv = self.R[sp][:, q0:q1, k0:k1]
            for i, e in enumerate(ENG):
                if e == eng:
                    continue
                need[e] = max(need[e], int(Wv[i].max()), int(Rv[i].max()))
            dmas.update(np.unique(self.Wd[sp][q0:q1, k0:k1]).tolist())
            dmas.update(np.unique(self.Rd[sp][q0:q1, k0:k1]).tolist())
        if eng == "pe":
            need["pe"] = 0
        for e in ENG:
            if need[e] > 0:
                self._wait_eng(eng, e, need[e])
        for d in sorted(dmas):
            self._wait_dma(eng, int(d))

    def op(self, eng, fn, reads, writes, inc=True):
        rr = [r for r in (self.region(a) for a in reads) if r is not None]
        ww = [r for r in (self.region(a) for a in writes) if r is not None]
        self.sync(eng, rr, ww)
        ins = fn()
        if inc:
            self.cnt[eng] += 1
            ins.then_inc(self.sem[eng], 1)
            t = self.cnt[eng]
        else:
            t = self.cnt[eng] + 1
        i = ENG.index(eng)
        for (sp, q0, q1, k0, k1) in rr:
            self.R[sp][i, q0:q1, k0:k1] = t
        for (sp, q0, q1, k0, k1) in ww:
            self.W[sp][i, q0:q1, k0:k1] = t

    def dma(self, out, in_, queue="sp"):
        rr = [r for r in (self.region(in_),) if r is not None]
        ww = [r for r in (self.region(out),) if r is not None]
        self.sync(queue, rr, ww)
        self.ndma += 1
        d = self.ndma
        if d > self.NDS:
            self._wait_dma(queue, d - self.NDS)
        s = (d - 1) % self.NDS
        self.h[queue].dma_start(out=out, in_=in_).then_inc(self.dsem[s], 16)
        for (sp, q0, q1, k0, k1) in rr:
            self.Rd[sp][q0:q1, k0:k1] = d
        for (sp, q0, q1, k0, k1) in ww:
            self.Wd[sp][q0:q1, k0:k1] = d

    def finish(self):
        for d in range(max(1, self.ndma - self.NDS + 1), self.ndma + 1):
            self._wait_dma("sp", d)


class Builder:
    def __init__(self, nc, dbg=None):
        self.nc = nc
        self.dbg = dbg
        self.sc = Sched(nc)
        self.big = nc.alloc_sbuf_tensor_at("big", [128, CAP], U8, offset=SB_BASE)
        self.psum = nc.alloc_psum_tensor("psum", [128, 4096], F32)
        self.stg_i = 0
        self.ev_i = 0
        self.cast_eng = "dve"
        self.deferred = []
        self.l1_pre = False

    def sb(self, off, shape, dt, parts=128, p0=0):
        e = _esz(dt)
        n = int(np.prod(shape))
        assert off % 4 == 0 and off + n * e <= CAP, (off, shape)
        v = self.big[p0:p0 + parts, off:off + n * e].bitcast(dt)
        if len(shape) == 2:
            v = v.rearrange("p (a b) -> p a b", b=shape[1])
        elif len(shape) == 3:
            v = v.rearrange("p (a b c) -> p a b c", b=shape[1], c=shape[2])
        return v

    def bank(self, i, parts=128):
        return self.psum[0:parts, 512 * i:512 * (i + 1)]

    def bankbf(self, i):
        return self.psum[:, 512 * i:512 * (i + 1)].bitcast(BF16)[:, 0:512]

    def mm(self, out, lhsT, rhs, start, stop, inc=None):
        if inc is None:
            inc = stop
        nc = self.nc
        self.sc.op("pe", lambda: nc.tensor.matmul(out, lhsT, rhs, start=start, stop=stop, skip_group_check=True),
                   [lhsT, rhs], [out], inc=inc)

    def tr(self, out, in_, ident, inc):
        nc = self.nc
        self.sc.op("pe", lambda: nc.tensor.transpose(out, in_, ident), [in_, ident], [out], inc=inc)

    def act(self, out, in_, func, scale=1.0, bias=0.0, eng="act"):
        nc = self.nc
        reads = [in_]
        if not isinstance(scale, float):
            reads.append(scale)
        if not isinstance(bias, float):
            reads.append(bias)
        self.sc.op("act", lambda: nc.scalar.activation(out=out, in_=in_, func=func, bias=bias, scale=scale),
                   reads, [out])

    def copy(self, eng, out, in_):
        nc = self.nc
        if eng == "act":
            self.sc.op("act", lambda: nc.scalar.copy(out, in_), [in_], [out])
        elif eng == "dve":
            self.sc.op("dve", lambda: nc.vector.tensor_copy(out, in_), [in_], [out])
        else:
            self.sc.op("pool", lambda: nc.gpsimd.tensor_copy(out, in_), [in_], [out])

    def tt(self, out, in0, in1, op, eng="dve"):
        h = self.sc.h[eng]
        self.sc.op(eng, lambda: h.tensor_tensor(out, in0, in1, op), [in0, in1], [out])

    def ts(self, out, in0, s1, s2, op0, op1=None, eng="dve"):
        h = self.sc.h[eng]
        reads = [in0] + [s for s in (s1, s2) if s is not None and not isinstance(s, float)]
        if op1 is None:
            self.sc.op(eng, lambda: h.tensor_scalar(out, in0, s1, None, op0), reads, [out])
        else:
            self.sc.op(eng, lambda: h.tensor_scalar(out, in0, s1, s2, op0, op1), reads, [out])

    def stt(self, out, in0, scalar, in1, op0, op1, eng="dve"):
        h = self.sc.h[eng]
        reads = [in0, in1] + ([] if isinstance(scalar, float) else [scalar])
        self.sc.op(eng, lambda: h.scalar_tensor_tensor(out, in0, scalar, in1, op0, op1), reads, [out])

    def red(self, out, in_, op, eng="dve"):
        h = self.sc.h[eng]
        self.sc.op(eng, lambda: h.tensor_reduce(out, in_, AX.X, op), [in_], [out])

    def recip(self, out, in_):
        nc = self.nc
        self.sc.op("dve", lambda: nc.vector.reciprocal(out, in_), [in_], [out])

    def memset(self, eng, out, val):
        h = self.sc.h[eng]
        self.sc.op(eng, lambda: h.memset(out, val), [], [out])

    def dma(self, out, in_, queue="sp"):
        self.sc.dma(out, in_, queue)

    def load_w(self, dram, dst, pk=128, cast=None, maxn=1024):
        K, N = dram.shape
        self.dma(dst[0:pk, 0:K // pk, 0:N], dram.rearrange("(kc p) n -> p kc n", p=pk), queue="pool")

    def tok2feat(self, banks=(0, 1), scale=1.0, Ts=(0, 1, 2, 3)):
        for T in Ts:
            for kc in range(8):
                bk = self.bank(banks[self.ev_i % len(banks)])
                for j in range(4):
                    t = 4 * T + j
                    self.tr(bk[:, j * 128:(j + 1) * 128], self.XRES[:, t, kc * 128:(kc + 1) * 128], self.IDENT,
                            inc=(j == 3))
                eng = "act" if (self.ev_i % 2 == 0) else "dve"
                self.ev_i += 1
                dst = self.XT[:, kc, T * 512:(T + 1) * 512]
                if scale == 1.0:
                    self.copy(eng, dst, bk)
                elif eng == "act":
                    self.act(dst, bk, AF.Copy, scale=float(scale))
                else:
                    self.ts(dst, bk, float(scale), None, ALU.mult)

    def ln_stats(self, t, src_chunks):
        nc = self.nc
        st = self.BNST[:, t % 2, :]
        for c, src in enumerate(src_chunks):
            dst = st[:, c * 6:(c + 1) * 6]
            self.sc.op("dve", lambda s_=src, d_=dst: nc.vector.bn_stats(d_, s_), [src], [dst])
        mv = self.MVALL[:, t, :]
        self.sc.op("dve", lambda: nc.vector.bn_aggr(mv, st), [st], [mv])

    def xres_stats(self, t):
        self.ln_stats(t, [self.XRES[:, t, c * 512:(c + 1) * 512] for c in range(2)])

    def ln_rstd(self):
        self.ts(self.VE, self.MVALL[:, :, 1], LN_EPS, None, ALU.add)
        self.act(self.VE, self.VE, AF.Sqrt)
        self.recip(self.RSTD, self.VE)

    def ln_load(self, g_dram, b_dram, oscale=1.0):
        self.dma(self.LNG, g_dram.partition_broadcast(128))
        self.dma(self.LNB, b_dram.partition_broadcast(128))
        if oscale != 1.0:
            self.ts(self.LNG, self.LNG, float(oscale), None, ALU.mult)
            self.ts(self.LNB, self.LNB, float(oscale), None, ALU.mult)

    def ln_block(self, g_dram, b_dram, oscale, make_xt, gF=None, bF=None, stats_done=False):
        self.ln_load(g_dram, b_dram, oscale)
        if make_xt:
            self.dma(self.GF, gF)
            self.dma(self.BF, bF)
        if not stats_done:
            for t in range(NT):
                self.xres_stats(t)
        self.ln_rstd()
        self.stt(self.NMR, self.MVALL[:, :, 0], -1.0, self.RSTD, ALU.mult, ALU.mult)

        def norm(T):
            for t in range(4 * T, 4 * T + 4):
                x = self.XRES[:, t, :]
                if t % 3 == 2:
                    self.ts(x, x, self.MVALL[:, t, 0:1], self.RSTD[:, t:t + 1], ALU.subtract, ALU.mult)
                else:
                    self.act(x, x, AF.Identity, scale=self.RSTD[:, t:t + 1], bias=self.NMR[:, t:t + 1])

        def affine(t):
            x = self.XRES[:, t, :]
            self.tt(x, x, self.LNG, ALU.mult)
            self.tt(x, x, self.LNB, ALU.add)

        def xpose(T):
            for kc in range(8):
                bk = self.bank(self.ev_i % 2)
                self.ev_i += 1
                for j in range(4):
                    self.tr(bk[:, j * 128:(j + 1) * 128], self.XRES[:, 4 * T + j, kc * 128:(kc + 1) * 128], self.IDENT,
                            inc=(j == 3))
                if kc % 2 == 0:
                    self.ts(self.XT[:, kc, T * 512:(T + 1) * 512], bk, self.GF[:, kc:kc + 1], self.BF[:, kc:kc + 1],
                            ALU.mult, ALU.add)
                else:
                    self.act(self.XT[:, kc, T * 512:(T + 1) * 512], bk, AF.Identity, scale=self.GF[:, kc:kc + 1],
                             bias=self.BF[:, kc:kc + 1])
        if not make_xt:
            for T in range(4):
                norm(T)
                for t in range(4 * T, 4 * T + 4):
                    affine(t)
            return
        norm(0)
        for T in range(4):
            if T + 1 < 4:
                norm(T + 1)
            xpose(T)
        for t in range(NT):
            self.deferred.append(lambda t=t: affine(t))

    def mem_kv(self, memT, wk, wv, wkv2=None):
        self.load_w(memT, self.MEMT)
        self.load_w(wk, self.WKV, maxn=256)
        wv_buf = wkv2 if wkv2 is not None else self.WKV
        if wkv2 is not None:
            self.load_w(wv, wv_buf, maxn=256)
        for hp in range(2):
            bk = self.bank(hp)
            for kc in range(8):
                self.mm(bk[:, 0:256], self.WKV[:, kc, hp * 128:(hp + 1) * 128], self.MEMT[:, kc, :], kc == 0, kc == 7)
            self.copy("dve", self.MKT[:, hp, :], bk[:, 0:256])
        if wkv2 is None:
            self.load_w(wv, wv_buf, maxn=256)
        self.memset("dve", self.MV[:, :, :, 64:65], 1.0)
        for mc in range(2):
            bk = self.bank(2 + mc)
            for kc in range(8):
                self.mm(bk[:, 0:256], self.MEMT[:, kc, mc * 128:(mc + 1) * 128], wv_buf[:, kc, :], kc == 0, kc == 7)
            self.copy("dve", self.MV[:, mc, :, 0:64], bk[:, 0:256].rearrange("p (h d) -> p h d", d=64))

    def mem_attention(self):
        units = [(h, T) for h in range(4) for T in range(4)]

        def stage1(it):
            h, T = units[it]
            hp, po = h // 2, (h % 2) * 64
            sb0 = 0 + 2 * (it % 2)
            pt = self.PT[it % 2]
            for mc in range(2):
                self.mm(self.bank(sb0 + mc), self.MKT[po:po + 64, hp, mc * 128:(mc + 1) * 128],
                        self.QMT[po:po + 64, hp, T * 512:(T + 1) * 512], True, True)
                self.act(pt[:, mc, :], self.bank(sb0 + mc), AF.Exp, scale=0.125)

        def stage2(it):
            h, T = units[it]
            ob = 4 + 2 * (it % 2)
            pt = self.PT[it % 2]
            bo = self.bank(ob, 65)
            bd = self.bank(ob + 1, 64)
            for mc in range(2):
                self.mm(bo, self.MV[:, mc, h, :], pt[:, mc, :], mc == 0, mc == 1)
            self.copy("act", self.DROW, bo[64:65, :])
            pend.append((bo, bd, self.REC[it % 2], self.MOT[0:64, h, T * 512:(T + 1) * 512]))

        def fin():
            while pend:
                bo, bd, rc, dst = pend.pop(0)
                self.mm(bd, self.ONESF[64:65, :], self.DROW, True, True)
                self.recip(rc, bd)
                self.tt(dst, bo[0:64, :], rc, ALU.mult)
        pend = []
        for it in range(len(units) + 1):
            if it < len(units):
                stage1(it)
            fin()
            if it >= 1:
                stage2(it - 1)
        fin()

    def finish_softmax(self, bo, bd, rc, dst):
        self.copy("act", self.DROW, bo[64:65, :])
        self.mm(bd, self.ONESF[64:65, :], self.DROW, True, True)
        self.recip(rc, bd)
        self.tt(dst, bo[0:64, :], rc, ALU.mult)

    def out_proj_res(self, chunks, prescaled):
        for t in range(NT):
            for nh in range(2):
                bk = self.bank((2 * t + nh) % 4)
                n = len(chunks)
                for i, (lf, wf) in enumerate(chunks):
                    self.mm(bk, lf(t), wf(nh), i == 0, i == n - 1)
                x = self.XRES[:, t, nh * 512:(nh + 1) * 512]
                if prescaled:
                    self.tt(x, bk, x, ALU.add)
                else:
                    self.stt(x, x, ALPHA, bk, ALU.mult, ALU.add)
            self.xres_stats(t)

    def routing_load(self, wr_dram, br_dram):
        self.load_w(wr_dram, self.WRT, maxn=16, cast="dve")
        self.dma(self.BRT, br_dram.partition_broadcast(128))

    def routing(self):
        nc = self.nc
        bk = self.bank(7)
        for t in range(NT):
            for kc in range(8):
                self.mm(bk[:, t * 16:(t + 1) * 16], self.XT[:, kc, t * 128:(t + 1) * 128], self.WRT[:, kc, :],
                        kc == 0, kc == 7)
        RT = self.RT
        L3 = bk[:, 0:256].rearrange("p (t e) -> p t e", e=16)

        def v3(i):
            return RT[:, i, :].rearrange("p (t e) -> p t e", e=16)

        def v4(i):
            return RT[:, i, :].rearrange("p (a b) -> p a b", b=4)
        mx = self.RS[:, 0, :]
        self.red(mx, L3, ALU.max)
        self.tt(v3(0), L3, mx.unsqueeze(2).to_broadcast([128, 16, 16]), ALU.subtract)
        self.act(RT[:, 1, :], RT[:, 0, :], AF.Exp)
        sm = self.RS[:, 1, :]
        self.red(sm, v3(1), ALU.add)
        rs = self.RS[:, 2, :]
        self.recip(rs, sm)
        self.tt(v3(2), v3(1), rs.unsqueeze(2).to_broadcast([128, 16, 16]), ALU.mult)
        self.tt(v3(3), v3(2), self.BRT.unsqueeze(1).to_broadcast([128, 16, 16]), ALU.add)
        S4 = v4(3)
        PR = self.RT6
        self.tt(PR[:, :, 0:3], S4[:, :, 0:3], S4[:, :, 1:4], ALU.add)
        self.tt(PR[:, :, 3:5], S4[:, :, 0:2], S4[:, :, 2:4], ALU.add)
        self.tt(PR[:, :, 5:6], S4[:, :, 0:1], S4[:, :, 3:4], ALU.add)
        GS = self.RS[:, 3:7, :].rearrange("p a b -> p (a b)")
        self.red(GS, PR, ALU.max)
        gmx = self.RS[:, 7, :]
        self.red(gmx, GS.rearrange("p (t g) -> p t g", g=4), ALU.max)
        GM = self.RS[:, 8:12, :].rearrange("p a b -> p (a b)").rearrange("p (t g) -> p t g", g=4)
        self.tt(GM, GS.rearrange("p (t g) -> p t g", g=4), gmx.unsqueeze(2).to_broadcast([128, 16, 4]), ALU.is_ge)
        for j in range(4):
            self.tt(v4(4 + j) if j < 3 else v4(7), S4[:, :, j:j + 1].to_broadcast([128, 64, 4]), S4, ALU.is_gt)
        self.tt(v4(4), v4(4), v4(5), ALU.add)
        self.tt(v4(6), v4(6), v4(7), ALU.add)
        self.tt(v4(4), v4(4), v4(6), ALU.add)
        self.ts(v4(5), v4(4), 1.5, None, ALU.is_lt)
        self.tt(v4(6), v4(5), GM.rearrange("p t g -> p (t g)").unsqueeze(2).to_broadcast([128, 64, 4]), ALU.mult)
        self.tt(RT[:, 7, :], RT[:, 2, :], RT[:, 6, :], ALU.mult)
        g2 = self.RS[:, 12, :]
        self.red(g2, v3(7), ALU.add)
        rg = self.RS[:, 13, :]
        self.recip(rg, g2)
        self.tt(self.GATES, v3(7), rg.unsqueeze(2).to_broadcast([128, 16, 16]), ALU.mult)

    def moe_load(self, wg, wu, wd, e):
        ring = self.WRING
        for m, w in enumerate((wg, wu, wd)):
            self.load_w(w[e], ring[(3 * e + m + 1) % 4], cast="pool")

    def moe(self, wg, wu, wd, hook=None):
        ring = self.WRING
        gu_i = 0
        dn_i = 0
        for e in range(16):
            Wg = ring[(3 * e + 1) % 4]
            Wu = ring[(3 * e + 2) % 4]
            Wd = ring[(3 * e + 3) % 4]
            if e > 0:
                self.moe_load(wg, wu, wd, e)
            if e == 15 and hook is not None:
                hook()
            for T in range(4):
                for fc in range(8):
                    bg = self.bank(0 + 2 * (gu_i % 2))
                    bu = self.bank(1 + 2 * (gu_i % 2))
                    sg = self.SG[gu_i % 2]
                    gu_i += 1
                    for kc in range(8):
                        self.mm(bg, Wg[:, kc, fc * 128:(fc + 1) * 128], self.XT[:, kc, T * 512:(T + 1) * 512],
                                kc == 0, kc == 7)
                    for kc in range(8):
                        self.mm(bu, Wu[:, kc, fc * 128:(fc + 1) * 128], self.XT[:, kc, T * 512:(T + 1) * 512],
                                kc == 0, kc == 7)
                    self.act(sg, bg, AF.Silu)
                    self.tt(self.ACTT[:, fc, T * 512:(T + 1) * 512], sg, bu, ALU.mult)
                    self.drain(1)
            self.drain_all()
            for t in range(NT):
                b0 = 4 + 2 * (dn_i % 2)
                dn_i += 1
                for nh in range(2):
                    bk = self.bank(b0 + nh)
                    for fc in range(8):
                        self.mm(bk, self.ACTT[:, fc, t * 128:(t + 1) * 128], Wd[:, fc, nh * 512:(nh + 1) * 512],
                                fc == 0, fc == 7)
                y = self.XRES[:, t, :]
                pv = self.psum[:, 512 * b0:512 * (b0 + 2)]
                self.stt(y, pv, self.GATES[:, t, e:e + 1], y, ALU.mult, ALU.add)
                if e == 15:
                    self.xres_stats(t)

    def drain(self, n=1):
        for _ in range(n):
            if self.deferred:
                self.deferred.pop(0)()

    def drain_all(self):
        while self.deferred:
            self.deferred.pop(0)()

    def l1_prefetch(self):
        I = self.I
        self.MEMT = self.sb(self.STG_OFF[0], [8, 256], BF16)
        self.WKV = self.sb(self.STG_OFF[1], [8, 256], BF16)
        self.mem_kv(I["memT"], I["w_mem_k"][1], I["w_mem_v"][1])
        for i in range(2):
            self.load_w(I["w_in_b"][:, i * 256:(i + 1) * 256], self.sb(self.STG_OFF[i], [8, 256], BF16), maxn=256)
        self.l1_pre = True

    def dump(self, dbg_out):
        self.drain_all()
        for t in range(NT):
            self.dma(dbg_out[t * 128:(t + 1) * 128, :], self.XRES[:, t, :])

    def build(self, I, out):
        nc = self.nc
        self.I = I
        o = 0

        def take(n):
            nonlocal o
            r = o
            o += (n + 63) // 64 * 64
            assert o <= CAP, o
            return r
        self.XRES = self.sb(take(65536), [16, 1024], F32)
        XT_OFF = take(32768)
        self.XT = self.sb(XT_OFF, [8, 2048], BF16)
        self.IDENT = self.sb(take(512), [128], F32)
        self.IDB = self.sb(take(256), [128], BF16)
        STG0_OFF = take(4096)
        STG1_OFF = take(4096)
        self.STG = [self.sb(STG0_OFF, [1024], F32), self.sb(STG1_OFF, [1024], F32)]
        self.STG_OFF = [STG0_OFF, STG1_OFF]
        self.GATES = self.sb(take(1024), [16, 16], F32)
        self.DROW = self.big[64:65, STG1_OFF:STG1_OFF + 2048].bitcast(F32)
        self.MVALL = self.sb(take(128), [16, 2], F32)
        self.VE = self.sb(take(64), [16], F32)
        self.RSTD = self.sb(take(64), [16], F32)
        self.NMR = self.sb(take(64), [16], F32)
        self.GF = self.sb(take(32), [8], F32)
        self.BF = self.sb(take(32), [8], F32)
        self.ONESF = self.sb(take(256), [64], F32)
        self.BNST = self.sb(take(96), [2, 12], F32)
        self.ONES = self.sb(take(128), [64], BF16)
        self.MKT = self.sb(take(1024), [2, 256], BF16)
        self.MV = self.sb(take(1040), [2, 4, 65], BF16)
        LN_OFF = take(4096)
        self.LNG = self.sb(LN_OFF, [1024], F32)
        self.LNB = self.sb(take(4096), [1024], F32)
        PH = o
        PHSZ = CAP - PH

        self.dma(self.IDENT, I["ident"])
        self.copy("dve", self.IDB, self.IDENT)
        self.memset("dve", self.ONES, 1.0)
        self.memset("dve", self.ONESF, 1.0)
        for t in range(0, NT, 4):
            self.dma(self.XRES[:, t:t + 4, :], I["x"][t * 128:(t + 4) * 128, :].rearrange("(a p) d -> p a d", p=128))

        A0 = PH
        B0 = PH + 28672
        C0 = PH + 53248
        D0 = PH + 77824
        E0 = PH + 86016
        WIN = self.sb(A0, [8, 1792], BF16)
        MIXT = self.sb(A0, [6, 2048], BF16)
        U = self.sb(B0, [16, 768], BF16)
        WOA = self.sb(B0, [6, 1024], BF16)
        WOM = self.sb(B0 + 12288, [4, 1024], BF16)
        GV = self.sb(C0, [16, 768], BF16)
        self.MOT = self.sb(C0, [4, 2048], BF16)
        self.PT = [self.sb(C0 + 16384 + i * 2048, [2, 512], BF16) for i in range(2)]
        self.REC = [self.sb(C0 + 20480 + i * 2048, [512], F32, parts=64) for i in range(2)]
        self.QMT = self.sb(D0, [2, 2048], BF16)
        SGW = self.sb(E0, [12, 128], BF16)
        BST = self.sb(E0 + 3072, [12], F32)
        TRIL = self.sb(E0 + 3072 + 256, [128], F32)
        self.MEMT = self.sb(B0, [8, 256], BF16)
        self.WKV = self.sb(B0 + 4096, [8, 256], BF16)
        assert E0 + 3072 + 256 + 512 <= CAP

        self.mem_kv(I["memT"], I["w_mem_k"][0], I["w_mem_v"][0], wkv2=self.sb(B0 + 8192, [8, 256], BF16))
        self.load_w(I["w_in_a"], WIN)
        self.dma(TRIL, I["tril"])
        self.dma(BST, I["sg_bT"])
        self.dma(self.LNG[:, 0:768], I["sg_ln_g"].partition_broadcast(128))
        self.dma(self.LNB[:, 0:768], I["sg_ln_b"].partition_broadcast(128))

        for T in range(4):
            self.tok2feat(Ts=(T,))
            for t in range(4 * T, 4 * T + 4):
                bs = [2, 3, 4] if t % 2 == 0 else [5, 6, 7]
                for nb in range(3):
                    for kc in range(8):
                        self.mm(self.bank(bs[nb]), self.XT[:, kc, t * 128:(t + 1) * 128],
                                WIN[:, kc, nb * 512:(nb + 1) * 512], kc == 0, kc == 7)
                gvt = self.STG[t % 2]
                self.act(U[:, t, 0:512], self.bank(bs[0]), AF.Gelu)
                self.act(U[:, t, 512:768], self.bank(bs[1])[:, 0:256], AF.Gelu)
                self.act(gvt[:, 0:256], self.bank(bs[1])[:, 256:512], AF.Gelu)
                self.act(gvt[:, 256:768], self.bank(bs[2]), AF.Gelu)
                self.ln_stats(t, [gvt[:, c * 384:(c + 1) * 384] for c in range(2)])
                self.copy("pool", GV[:, t, :], gvt[:, 0:768])
        for i in range(2):
            st = self.STG[self.stg_i % 2]
            self.stg_i += 1
            stv = st[:, 0:768].rearrange("p (g q) -> p g q", q=128)
            self.dma(st[:, 0:768], I["sg_wT"][:, i * 768:(i + 1) * 768])
            self.tt(SGW[:, 6 * i:6 * i + 6, :], stv, TRIL.unsqueeze(1).to_broadcast([128, 6, 128]), ALU.mult)
        i = 0
        for hp in range(2):
            for T in range(4):
                bk = self.bank(i % 2)
                i += 1
                for kc in range(8):
                    self.mm(bk, WIN[:, kc, 1536 + hp * 128:1536 + (hp + 1) * 128], self.XT[:, kc, T * 512:(T + 1) * 512],
                            kc == 0, kc == 7)
                self.copy("act", self.QMT[:, hp, T * 512:(T + 1) * 512], bk)
        self.ln_rstd()
        self.stt(self.NMR, self.MVALL[:, :, 0], -1.0, self.RSTD, ALU.mult, ALU.mult)
        for t in range(NT):
            gvt = self.STG[t % 2][:, 0:768]
            self.act(gvt, GV[:, t, :], AF.Identity, scale=self.RSTD[:, t:t + 1], bias=self.NMR[:, t:t + 1])
            self.tt(gvt, gvt, self.LNG[:, 0:768], ALU.mult)
            self.tt(GV[:, t, :], gvt, self.LNB[:, 0:768], ALU.add)
        i = 0
        for hf in range(2):
            for g in range(12):
                bk = self.bank(2 + i % 4)
                i += 1
                bk3 = bk.rearrange("p (c d) -> p c d", d=64)
                self.mm(bk3, SGW[:, g, :], GV[:, 8 * hf:8 * hf + 8, g * 64:(g + 1) * 64], True, True)
                uu = U[:, 8 * hf:8 * hf + 8, g * 64:(g + 1) * 64]
                ts_ = self.STG[i % 2][:, 0:512]
                self.act(ts_, bk, AF.Identity, bias=BST[:, g:g + 1])
                self.tt(uu, uu, ts_.rearrange("p (c d) -> p c d", d=64), ALU.mult)
        i = 0
        for fc in range(6):
            for T in range(4):
                b = i % 2
                i += 1
                bb = self.bankbf(b)
                for j in range(4):
                    self.tr(bb[:, j * 128:(j + 1) * 128], U[:, 4 * T + j, fc * 128:(fc + 1) * 128], self.IDB, inc=(j == 3))
                self.copy("act", MIXT[:, fc, T * 512:(T + 1) * 512], bb)
        self.load_w(I["w_out_a"][0:768, :], WOA)
        self.load_w(I["w_out_a"][768:1024, :], WOM, pk=64)
        self.mem_attention()
        chunks = [(lambda t, fc=fc: MIXT[:, fc, t * 128:(t + 1) * 128], lambda nh, fc=fc: WOA[:, fc, nh * 512:(nh + 1) * 512])
                  for fc in range(6)]
        chunks += [(lambda t, h=h: self.MOT[0:64, h, t * 128:(t + 1) * 128], lambda nh, h=h: WOM[0:64, h, nh * 512:(nh + 1) * 512])
                   for h in range(4)]
        self.out_proj_res(chunks, prescaled=False)
        if self.dbg == "r1_0":
            return self.dump(out)
        self.ffn_block(I, 0, LN_OFF)
        if self.dbg in ("ln1_0", "r2_0", "gates_0"):
            return self.dump(out)
        if self.dbg == "l0":
            return self.dump(out)

        QT = self.sb(PH, [6, 2048], BF16)
        KT = self.sb(PH + 24576, [6, 2048], BF16)
        V = self.sb(PH + 49152, [48, 4, 65], BF16)
        self.QMT = self.sb(PH + 74240, [2, 2048], BF16)
        W1O = PH + 82432
        W1 = [self.sb(self.STG_OFF[i], [8, 256], BF16) for i in range(2)]
        BASEA = self.sb(W1O + 8192, [128], F32)
        BASEB = self.sb(W1O + 8192 + 512, [128], F32)
        assert W1O + 8192 + 1024 <= CAP
        if not self.l1_pre:
            self.MEMT = W1[0]
            self.WKV = W1[1]
            self.mem_kv(I["memT"], I["w_mem_k"][1], I["w_mem_v"][1])
        self.dma(BASEA, I["baseA"])
        self.dma(BASEB, I["baseB"])
        wi = 0
        bi = 0
        for (wsrc, c0, dst, npair) in ((I["w_in_b"], 0, QT, 6), (I["w_k"], 0, KT, 6), (I["w_in_b"], 768, self.QMT, 2)):
            for pp in range(0, npair, 2):
                w = W1[wi % 2]
                if not (self.l1_pre and wi < 2):
                    self.load_w(wsrc[:, c0 + pp * 128:c0 + (pp + 2) * 128], w, maxn=256)
                wi += 1
                for p2 in range(2):
                    for T in range(4):
                        bk = self.bank(bi % 2)
                        bi += 1
                        for kc in range(8):
                            self.mm(bk, w[:, kc, p2 * 128:(p2 + 1) * 128], self.XT[:, kc, T * 512:(T + 1) * 512],
                                    kc == 0, kc == 7)
                        self.copy("act" if bi % 2 else "dve", dst[:, pp + p2, T * 512:(T + 1) * 512], bk)
                        if bi % 2 == 0:
                            self.drain(1)
        self.memset("dve", V[:, :, :, 64:65], 1.0)
        for g in range(3):
            w = W1[wi % 2]
            wi += 1
            self.load_w(I["w_v"][:, g * 256:(g + 1) * 256], w, maxn=256)
            for idx in range(16):
                if g == 0:
                    tok = slice(idx * 128, (idx + 1) * 128)
                elif g == 1:
                    t0 = (idx // 4) + 512 * (idx % 4)
                    tok = slice(t0, t0 + 4 * 127 + 1, 4)
                else:
                    tok = slice(idx, idx + 16 * 127 + 1, 16)
                bk = self.bank(2 + (bi % 2))
                bi += 1
                for kc in range(8):
                    self.mm(bk[:, 0:256], self.XT[:, kc, tok], w[:, kc, :], kc == 0, kc == 7)
                self.copy("act" if bi % 2 else "dve", V[:, g * 16 + idx, :, 0:64],
                          bk[:, 0:256].rearrange("p (h d) -> p h d", d=64))
                if bi % 4 == 0:
                    self.drain(1)
        self.drain_all()
        MIXB = self.sb(XT_OFF, [4, 2048], BF16)
        self.MOT = self.sb(XT_OFF + 16384, [4, 2048], BF16)
        NB = 4
        TMP = [self.sb(W1O + i * 2048, [512], F32) for i in range(NB)]
        PTD = [self.sb(STG0_OFF + i * 1024, [512], BF16) for i in range(NB)]
        RECD = self.sb(STG1_OFF + 2048, [512], F32, parts=64)
        slopes = [2.0 ** (-8.0 * (h + 1) / 12.0) for h in range(12)]
        dil = [1, 4, 16]

        def sl(start, n, step=1):
            return slice(start, start + step * (n - 1) + 1, step)
        units = []
        ni = 0
        for j in range(4):
            for Q in range(4):
                num = self.bank(4 + 2 * (ni % 2), 65)
                den = self.bank(5 + 2 * (ni % 2), 64)
                ni += 1
                batches = []
                for g in range(3):
                    h = 4 * g + j
                    hp, po = h // 2, (h % 2) * 64
                    c8 = 8.0 * slopes[h] * dil[g]
                    A, Bp = [], []
                    if g == 0:
                        for i4 in range(4):
                            qb = 4 * Q + i4
                            qtok = sl(qb * 128, 128)
                            A.append((KT[po:po + 64, hp, qtok], QT[po:po + 64, hp, qtok], i4 * 128, 128, qb,
                                      sl(i4 * 128, 128)))
                            if qb >= 1:
                                Bp.append((KT[po:po + 64, hp, sl((qb - 1) * 128, 128)], QT[po:po + 64, hp, qtok],
                                           i4 * 128, 128, qb - 1, sl(i4 * 128, 128)))
                        batches.append((BASEA, 128, c8, A))
                        batches.append((BASEB, 128, c8, Bp))
                    elif g == 1:
                        for r in range(4):
                            qtok = sl(512 * Q + r, 128, 4)
                            A.append((KT[po:po + 64, hp, qtok], QT[po:po + 64, hp, qtok], r * 128, 128,
                                      16 + r * 4 + Q, sl(r, 128, 4)))
                            if Q >= 1:
                                Bp.append((KT[po:po + 64, hp, sl(512 * (Q - 1) + r, 128, 4)], QT[po:po + 64, hp, qtok],
                                           r * 128, 128, 16 + r * 4 + Q - 1, sl(r, 128, 4)))
                        batches.append((BASEA, 128, c8, A))
                        if Bp:
                            batches.append((BASEB, 128, c8, Bp))
                    else:
                        for r in range(16):
                            A.append((KT[po:po + 64, hp, sl(r, 128, 16)], QT[po:po + 64, hp, sl(512 * Q + r, 32, 16)],
                                      r * 32, 32, 32 + r, sl(r, 32, 16)))
                        batches.append((BASEA[:, 32 * Q:32 * Q + 32], 32, c8, A[0:8]))
                        batches.append((BASEA[:, 32 * Q:32 * Q + 32], 32, c8, A[8:16]))
                total = sum(len(b_[3]) for b_ in batches)
                k_i = 0
                for bi_, (base, w_, c8, items) in enumerate(batches):
                    flags = []
                    for _ in items:
                        flags.append((k_i == 0, k_i == total - 1))
                        k_i += 1
                    units.append((j, Q, num, den, base, w_, c8, items, flags, bi_ == len(batches) - 1))

        def d_stage1(u, si):
            (j, Q, num, den, base, w_, c8, items, flags, lastb) = u
            sbk = self.bank(si % 4)
            tmp = TMP[si % NB]
            ptd = PTD[si % NB]
            c_lo = items[0][2]
            c_hi = items[-1][2] + items[-1][3]
            for ii, (lt, rt, c0, ncol, vb, oc) in enumerate(items):
                self.mm(sbk[:, c0:c0 + ncol], lt, rt, True, True, inc=(ii == len(items) - 1))
            nrep = (c_hi - c_lo) // w_
            self.stt(tmp[:, c_lo:c_hi].rearrange("p (a b) -> p a b", b=w_),
                     base.unsqueeze(1).to_broadcast([128, nrep, w_]), c8,
                     sbk[:, c_lo:c_hi].rearrange("p (a b) -> p a b", b=w_), ALU.mult, ALU.add)
            self.act(ptd[:, c_lo:c_hi], tmp[:, c_lo:c_hi], AF.Exp, scale=0.125)

        def d_stage2(u, si):
            (j, Q, num, den, base, w_, c8, items, flags, lastb) = u
            ptd = PTD[si % NB]
            n_it = len(items)
            for ii, ((lt, rt, c0, ncol, vb, oc), (first, last)) in enumerate(zip(items, flags)):
                self.mm(num[:, oc], V[:, vb, j, :], ptd[:, c0:c0 + ncol], first, last, inc=(ii == n_it - 1))
            if lastb:
                self.copy("act", self.DROW, num[64:65, :])
                pend_fin.append((num, den, j, Q))

        def d_finish():
            while pend_fin:
                num, den, j, Q = pend_fin.pop(0)
                self.mm(den, self.ONESF[64:65, :], self.DROW, True, True)
                self.recip(RECD, den)
                self.tt(MIXB[0:64, j, Q * 512:(Q + 1) * 512], num[0:64, :], RECD, ALU.mult)
        pend_fin = []
        LA = 3
        for si in range(len(units) + LA):
            if si < len(units):
                d_stage1(units[si], si)
            d_finish()
            if si >= LA:
                d_stage2(units[si - LA], si - LA)
        d_finish()
        WOB = self.sb(PH, [8, 1024], BF16)
        self.load_w(I["w_out_b"], WOB, pk=64)
        self.PT = [self.sb(PH + 24576 + i * 2048, [2, 512], BF16) for i in range(2)]
        self.REC = [self.sb(PH + 24576 + 4096 + i * 2048, [512], F32, parts=64) for i in range(2)]
        self.mem_attention()
        chunks = [(lambda t, jj=jj: MIXB[0:64, jj, t * 128:(t + 1) * 128], lambda nh, jj=jj: WOB[0:64, jj, nh * 512:(nh + 1) * 512])
                  for jj in range(4)]
        chunks += [(lambda t, h=h: self.MOT[0:64, h, t * 128:(t + 1) * 128], lambda nh, h=h: WOB[0:64, 4 + h, nh * 512:(nh + 1) * 512])
                   for h in range(4)]
        self.out_proj_res(chunks, prescaled=True)
        if self.dbg == "r1_1":
            return self.dump(out)
        self.ffn_block(I, 1, LN_OFF)
        self.dump(out)

    def ffn_block(self, I, l, LN_OFF):
        self.WRING = [self.sb(LN_OFF + i * 16384, [8, 1024], BF16) for i in range(4)]
        self.ACTT = self.sb(LN_OFF + 65536, [8, 2048], BF16)
        self.SG = [self.sb(LN_OFF + 98304 + i * 1024, [512], BF16) for i in range(2)]
        assert LN_OFF + 98304 + 2048 <= CAP
        A_OFF = LN_OFF + 65536
        self.RT = self.sb(A_OFF, [8, 256], F32)
        self.RT6 = self.sb(A_OFF + 8192, [64, 6], F32)
        self.RS = self.sb(A_OFF + 8192 + 1536, [14, 16], F32)
        self.WRT = self.sb(A_OFF + 12288, [8, 16], BF16)
        self.BRT = self.sb(A_OFF + 12288 + 256, [16], F32)
        wg, wu, wd = I["w_gate"][l], I["w_up"][l], I["w_down"][l]
        self.routing_load(I["w_router"], I["b_router"])
        if self.dbg is None or self.dbg in ("r2_%d" % l, "l%d" % l, "r1_1"):
            self.moe_load(wg, wu, wd, 0)
        self.ln_block(I["ln1_g"][l], I["ln1_b"][l], ALPHA, make_xt=True, gF=I["ln1_gF"][l], bF=I["ln1_bF"][l],
                      stats_done=True)
        if self.dbg == "ln1_%d" % l:
            return
        self.routing()
        if self.dbg == "gates_%d" % l:
            self.drain_all()
            for t in range(NT):
                self.copy("dve", self.XRES[:, t, 0:16], self.GATES[:, t, :])
            return
        self.moe(wg, wu, wd, hook=(self.l1_prefetch if (l == 0 and self.dbg is None) else None))
        if self.dbg == "r2_%d" % l:
            return
        if l == 0:
            self.ln_block(I["ln2_g"][l], I["ln2_b"][l], ALPHA, make_xt=True, gF=I["ln2_gF"][l], bF=I["ln2_bF"][l],
                          stats_done=True)
        else:
            self.ln_block(I["ln2_g"][l], I["ln2_b"][l], 1.0, make_xt=False, stats_done=True)


def _dram_inputs(nc):
    def t(name, shape):
        return nc.dram_tensor(name, list(shape), F32, kind="ExternalInput").ap()
    I = {}
    I["x"] = t("x", (S, D))
    I["memT"] = t("memT", (D, 256))
    I["w_in_a"] = t("w_in_a", (D, 1792))
    I["w_out_a"] = t("w_out_a", (1024, D))
    I["sg_ln_g"] = t("sg_ln_g", (768,))
    I["sg_ln_b"] = t("sg_ln_b", (768,))
    I["sg_wT"] = t("sg_wT", (128, 1536))
    I["sg_bT"] = t("sg_bT", (128, 12))
    I["w_in_b"] = t("w_in_b", (D, 1024))
    I["w_out_b"] = t("w_out_b", (512, D))
    I["w_k"] = t("w_k", (D, 768))
    I["w_v"] = t("w_v", (D, 768))
    I["w_mem_k"] = t("w_mem_k", (2, D, 256))
    I["w_mem_v"] = t("w_mem_v", (2, D, 256))
    for n in ("ln1_g", "ln1_b", "ln2_g", "ln2_b"):
        I[n] = t(n, (2, D))
        I[n + "F"] = t(n + "F", (2, 128, 8))
    I["w_router"] = t("w_router", (D, 16))
    I["b_router"] = t("b_router", (16,))
    for n in ("w_gate", "w_up", "w_down"):
        I[n] = t(n, (2, 16, D, D))
    I["ident"] = t("ident", (128, 128))
    I["tril"] = t("tril", (128, 128))
    I["baseA"] = t("baseA", (128, 128))
    I["baseB"] = t("baseB", (128, 128))
    return I


def build_program(dbg=None):
    nc = bass.Bass("TRN2", target_bir_lowering=False)
    I = _dram_inputs(nc)
    out = nc.dram_tensor("out", [S, D], F32, kind="ExternalOutput").ap()
    b = Builder(nc, dbg)
    b.build(I, out)
    b.sc.finish()
    return nc


def host_shared(inputs):
    f = lambda a: np.ascontiguousarray(np.asarray(a, dtype=np.float32))
    m = {}
    m["w_in_a"] = f(inputs["w_in_a"][0])
    m["w_out_a"] = f(inputs["w_out_a"][0])
    m["sg_ln_g"] = f(inputs["sg_ln_g"][0])
    m["sg_ln_b"] = f(inputs["sg_ln_b"][0])
    m["sg_wT"] = f(np.transpose(np.asarray(inputs["sg_w"][0]), (2, 0, 1)).reshape(128, 1536))
    m["sg_bT"] = f(np.asarray(inputs["sg_b"][0]).T)
    m["w_in_b"] = f(inputs["w_in_b"][0])
    m["w_out_b"] = f(inputs["w_out_b"][0])
    m["w_k"] = f(inputs["w_k_shared"])
    m["w_v"] = f(inputs["w_v_shared"])
    for n in ("w_mem_k", "w_mem_v", "ln1_g", "ln1_b", "ln2_g", "ln2_b", "w_router", "b_router",
              "w_gate", "w_up", "w_down"):
        m[n] = f(inputs[n])
    for n in ("ln1_g", "ln1_b", "ln2_g", "ln2_b"):
        m[n + "F"] = f(np.asarray(inputs[n]).reshape(2, 8, 128).transpose(0, 2, 1))
    k = np.arange(128)[:, None]
    q = np.arange(128)[None, :]
    m["ident"] = np.eye(128, dtype=np.float32)
    m["tril"] = (k <= q).astype(np.float32)
    relA = (q - k).astype(np.float32)
    m["baseA"] = np.where(q - k >= 0, -relA, NEG).astype(np.float32)
    relB = (128 + q - k).astype(np.float32)
    m["baseB"] = np.where(k >= q, -relB, NEG).astype(np.float32)
    return m


def host_inputs(inputs, b, shared=None):
    f = lambda a: np.ascontiguousarray(np.asarray(a, dtype=np.float32))
    m = dict(shared if shared is not None else host_shared(inputs))
    m["x"] = f(inputs["x"][b])
    m["memT"] = f(np.asarray(inputs["mem"][b]).T)
    return m


def kernel(**inputs):
    n = 8
    nc = build_program()
    shared = host_shared(inputs)
    in_maps = [host_inputs(inputs, b, shared) for b in range(n)]
    res = run_bass_kernel_spmd(nc, in_maps, core_ids=list(range(n)))
    return np.stack([np.asarray(r["out"], dtype=np.float32) for r in res.results], axis=0)
```
